# Optimizing a Trainium2 kernel written in Bass

```python
import math
import jax, jax.numpy as jnp
from jax import lax
import numpy as np

D_MODEL = 1024
BATCH = 32
SEQ = 2048
DEPTH = 1

MLA_HEADS = 8
MLA_Q_RANK = 256
MLA_KV_RANK = 256
MLA_NOPE = 64
MLA_ROPE = 32
MLA_V = 64
ROPE_BASE = 10000.0
DSA_HEADS = 8
DSA_HEAD_DIM = 64
IDX_HEADS = 8
IDX_DIM = 32
TOPK_MAX = 256
D_FF = int(math.ceil(8 * D_MODEL / 3 / 256)) * 256
ALPHA = (2 * DEPTH) ** 0.25
BETA = (8 * DEPTH) ** -0.25
LN_EPS = 1e-5
RMS_EPS = 1e-6
QBLOCK = 128

MLA_WIDTH = MLA_HEADS * MLA_V
DSA_WIDTH = DSA_HEADS * DSA_HEAD_DIM
SPLIT_SIZES = (
    MLA_Q_RANK,
    MLA_KV_RANK,
    MLA_ROPE,
    DSA_WIDTH,
    DSA_WIDTH,
    DSA_WIDTH,
    IDX_HEADS * IDX_DIM,
    IDX_DIM,
    IDX_HEADS,
    D_MODEL,
    D_MODEL,
)
D_IN = sum(SPLIT_SIZES)

kernel_name = "hybrid_mla_dsa_gated_deepnorm"


def split_cols(t):
    out = []
    start = 0
    for n in SPLIT_SIZES:
        out.append(t[..., start:start + n])
        start += n
    return out


def layer_norm(x, g, b):
    xf = x.astype(jnp.float32)
    mu = jnp.mean(xf, axis=-1, keepdims=True)
    var = jnp.mean(jnp.square(xf - mu), axis=-1, keepdims=True)
    y = (xf - mu) * lax.rsqrt(var + LN_EPS) * g.astype(jnp.float32) + b.astype(jnp.float32)
    return y.astype(x.dtype)


def rms_norm(x, g):
    xf = x.astype(jnp.float32)
    y = xf * lax.rsqrt(jnp.mean(jnp.square(xf), axis=-1, keepdims=True) + RMS_EPS)
    return (y * g.astype(jnp.float32)).astype(x.dtype)


def rope(x, pos):
    half = x.shape[-1] // 2
    inv_freq = ROPE_BASE ** (-jnp.arange(half, dtype=jnp.float32) / half)
    ang = pos.astype(jnp.float32)[:, :, None, None] * inv_freq
    cos, sin = jnp.cos(ang), jnp.sin(ang)
    xf = x.astype(jnp.float32)
    x1, x2 = xf[..., :half], xf[..., half:]
    return jnp.concatenate([x1 * cos - x2 * sin, x2 * cos + x1 * sin], axis=-1).astype(x.dtype)


def alibi_slopes(n):
    return jnp.asarray([2.0 ** (-8.0 * (i + 1) / n) for i in range(n)], dtype=jnp.float32)


def to_blocks(t):
    b, s = t.shape[0], t.shape[1]
    return jnp.moveaxis(t.reshape((b, s // QBLOCK, QBLOCK) + t.shape[2:]), 1, 0)


def from_blocks(t):
    t = jnp.moveaxis(t, 0, 1)
    return t.reshape((t.shape[0], t.shape[1] * t.shape[2]) + t.shape[3:])


def mla_attention(q_nope, q_rope, k_nope, k_rope, v, pos):
    scale = 1.0 / math.sqrt(MLA_NOPE + MLA_ROPE)

    def block(args):
        qn, qr, qp = args
        s = jnp.einsum('bqhd,bshd->bhqs', qn, k_nope) + jnp.einsum('bqhd,bsd->bhqs', qr, k_rope)
        s = s.astype(jnp.float32) * scale
        causal = pos[:, None, None, :] <= qp[:, None, :, None]
        s = jnp.where(causal, s, -jnp.inf)
        p = jax.nn.softmax(s, axis=-1).astype(v.dtype)
        return jnp.einsum('bhqs,bshd->bqhd', p, v)

    out = lax.map(block, (to_blocks(q_nope), to_blocks(q_rope), to_blocks(pos)))
    return from_blocks(out)


def dsa_attention(q, k, v, q_idx, k_idx, w_idx, pos):
    n_keys = k.shape[1]
    topk = min(TOPK_MAX, n_keys // 4)
    scale = 1.0 / math.sqrt(DSA_HEAD_DIM)
    idx_scale = 1.0 / math.sqrt(IDX_DIM * IDX_HEADS)
    slopes = alibi_slopes(DSA_HEADS)
    gather = jax.vmap(lambda table, ind: table[ind])

    def block(args):
        qb, qib, wb, qp = args
        dots = jnp.einsum('bqhd,bsd->bhqs', qib, k_idx).astype(jnp.float32)
        score = jnp.einsum('bqh,bhqs->bqs', wb.astype(jnp.float32), jax.nn.relu(dots)) * idx_scale
        causal = pos[:, None, :] <= qp[:, :, None]
        score = jnp.where(causal, score, -jnp.inf)
        _, sel = lax.top_k(score, topk)
        k_sel = gather(k, sel)
        v_sel = gather(v, sel)
        p_sel = gather(pos, sel)
        valid = p_sel <= qp[:, :, None]
        s = jnp.einsum('bqhd,bqkhd->bhqk', qb, k_sel).astype(jnp.float32) * scale
        dist = (qp[:, :, None] - p_sel).astype(jnp.float32)
        s = s - slopes[None, :, None, None] * dist[:, None]
        s = jnp.where(valid[:, None], s, -jnp.inf)
        p = jax.nn.softmax(s, axis=-1).astype(v.dtype)
        return jnp.einsum('bhqk,bqkhd->bqhd', p, v_sel)

    out = lax.map(block, (to_blocks(q), to_blocks(q_idx), to_blocks(w_idx), to_blocks(pos)))
    return from_blocks(out)


def setup_inputs(seed: int = 0) -> dict:
    key = jax.random.key(seed)
    ks = jax.random.split(key, 20)
    f32 = jnp.float32

    def nrm(k, shape, scale):
        return jax.random.normal(k, shape, f32) * scale

    L = DEPTH
    x = jax.random.normal(ks[0], (BATCH, SEQ, D_MODEL), f32)
    positions = jnp.broadcast_to(jnp.arange(SEQ, dtype=jnp.int32), (BATCH, SEQ))
    return {
        "x": x,
        "positions": positions,
        "w_in": nrm(ks[1], (L, D_MODEL, D_IN), D_MODEL ** -0.5),
        "mla_q_norm": 1.0 + nrm(ks[2], (L, MLA_Q_RANK), 0.01),
        "mla_kv_norm": 1.0 + nrm(ks[3], (L, MLA_KV_RANK), 0.01),
        "mla_w_uq": nrm(ks[4], (L, MLA_Q_RANK, MLA_HEADS * (MLA_NOPE + MLA_ROPE)), MLA_Q_RANK ** -0.5),
        "mla_w_ukv": nrm(ks[5], (L, MLA_KV_RANK, MLA_HEADS * (MLA_NOPE + MLA_V)), MLA_KV_RANK ** -0.5),
        "w_branch_a": nrm(ks[6], (L, MLA_WIDTH, D_MODEL), BETA * MLA_WIDTH ** -0.5),
        "w_branch_b": nrm(ks[7], (L, DSA_WIDTH, D_MODEL), BETA * DSA_WIDTH ** -0.5),
        "w_out": nrm(ks[8], (L, D_MODEL, D_MODEL), BETA * D_MODEL ** -0.5),
        "ln1_g": 1.0 + nrm(ks[9], (L, D_MODEL), 0.01),
        "ln1_b": nrm(ks[10], (L, D_MODEL), 0.01),
        "ffn_w_in": nrm(ks[11], (L, D_MODEL, 2 * D_FF), D_MODEL ** -0.5),
        "ffn_w_down": nrm(ks[12], (L, D_FF, D_MODEL), BETA * D_FF ** -0.5),
        "ln2_g": 1.0 + nrm(ks[13], (L, D_MODEL), 0.01),
        "ln2_b": nrm(ks[14], (L, D_MODEL), 0.01),
    }


def reference(x, positions, w_in, mla_q_norm, mla_kv_norm, mla_w_uq, mla_w_ukv,
              w_branch_a, w_branch_b, w_out, ln1_g, ln1_b, ffn_w_in, ffn_w_down,
              ln2_g, ln2_b):
    B, S, _ = x.shape
    h = x
    for l in range(DEPTH):
        proj = jnp.einsum('bsd,de->bse', h, w_in[l])
        c_q, c_kv, k_r, q_b, k_b, v_b, q_i, k_i, w_i, g_a, g_b = split_cols(proj)

        c_q = rms_norm(c_q, mla_q_norm[l])
        q_a = jnp.einsum('bsr,re->bse', c_q, mla_w_uq[l]).reshape(B, S, MLA_HEADS, MLA_NOPE + MLA_ROPE)
        q_nope = q_a[..., :MLA_NOPE]
        q_rope = rope(q_a[..., MLA_NOPE:], positions)
        c_kv = rms_norm(c_kv, mla_kv_norm[l])
        kv = jnp.einsum('bsr,re->bse', c_kv, mla_w_ukv[l]).reshape(B, S, MLA_HEADS, MLA_NOPE + MLA_V)
        k_nope, v_a = kv[..., :MLA_NOPE], kv[..., MLA_NOPE:]
        k_rope = rope(k_r[:, :, None, :], positions)[:, :, 0]
        o_a = mla_attention(q_nope, q_rope, k_nope, k_rope, v_a, positions).reshape(B, S, MLA_WIDTH)

        o_b = dsa_attention(
            q_b.reshape(B, S, DSA_HEADS, DSA_HEAD_DIM),
            k_b.reshape(B, S, DSA_HEADS, DSA_HEAD_DIM),
            v_b.reshape(B, S, DSA_HEADS, DSA_HEAD_DIM),
            q_i.reshape(B, S, IDX_HEADS, IDX_DIM), k_i, w_i, positions,
        ).reshape(B, S, DSA_WIDTH)

        y_a = jnp.einsum('bse,ed->bsd', o_a, w_branch_a[l])
        y_b = jnp.einsum('bse,ed->bsd', o_b, w_branch_b[l])
        mixed = jax.nn.sigmoid(g_a) * y_a + jax.nn.sigmoid(g_b) * y_b
        mix_out = jnp.einsum('bsd,de->bse', mixed, w_out[l])
        h = layer_norm(ALPHA * h + mix_out, ln1_g[l], ln1_b[l])

        gu = jnp.einsum('bsd,df->bsf', h, ffn_w_in[l])
        gate, up = gu[..., :D_FF], gu[..., D_FF:]
        f = jnp.einsum('bsf,fd->bsd', jax.nn.silu(gate) * up, ffn_w_down[l])
        h = layer_norm(ALPHA * h + f, ln2_g[l], ln2_b[l])
    return h
```

```python
import math
from contextlib import ExitStack
import numpy as np
import concourse.bass as bass
import concourse.mybir as mybir
from concourse.bass_utils import run_bass_kernel_spmd

F32 = mybir.dt.float32
BF16 = mybir.dt.bfloat16
I32 = mybir.dt.int32
ALU = mybir.AluOpType
AF = mybir.ActivationFunctionType
AX = mybir.AxisListType

D = 1024
S = 2048
NG = 4
GT = 512
DFF = 2816
NF = DFF // 128
DIN = 4424
ALPHA = 2.0 ** 0.25
LN_EPS = 1e-5
RMS_EPS = 1e-6
TOPK = 256
NIT = 17
SC_MLA = 1.0 / math.sqrt(96.0)
SC_DSA = 0.125
SLOPES = [2.0 ** (-(i + 1)) for i in range(8)]
C_CQ, C_CKV, C_KR, C_QB, C_KB, C_VB, C_QI, C_KI, C_WI, C_GA, C_GB = 0, 256, 512, 544, 1056, 1568, 2080, 2336, 2368, 2376, 3400
SEM_ROLL = 2000
SLABW = 4608


class Buf:
    __slots__ = ("name", "w", "r")

    def __init__(self, name):
        self.name = name
        self.w = None
        self.r = {}


class Eng:
    def __init__(self, name, eng):
        self.name = name
        self.eng = eng
        self.sem = None
        self.gen = 0
        self.n = 0
        self.seen = {}
        self.pending = False
        self.prog = []


class FW:
    def __init__(self, nc, stack):
        self.nc = nc
        self.stack = stack
        self.sems = {}
        self.E = {}
        self.dma_sem = {}

    def new_sem(self, name):
        self.sems[name] = self.stack.enter_context(self.nc.semaphore(name))
        return name

    def add_engine(self, name, eng):
        e = Eng(name, eng)
        e.sem = self.new_sem(f"p_{name}_0")
        self.E[name] = e

    def _collect(self, e, reads, writes):
        deps = {}
        for b in reads:
            if b.w is not None and deps.get(b.w[0], 0) < b.w[1]:
                deps[b.w[0]] = b.w[1]
        for b in writes:
            if b.w is not None and deps.get(b.w[0], 0) < b.w[1]:
                deps[b.w[0]] = b.w[1]
            for k, v in b.r.items():
                if deps.get(k, 0) < v:
                    deps[k] = v
        out = [(k, v) for k, v in deps.items() if e.seen.get(k, 0) < v and not (k == e.sem and v > e.n)
               and not (e.name == "tensor" and k.startswith("p_tensor"))]
        for k, v in out:
            e.seen[k] = v
        return out

    def _mark(self, d, reads, writes):
        for b in writes:
            b.w = d
            b.r = {}
        for b in reads:
            if b.r.get(d[0], 0) < d[1]:
                b.r[d[0]] = d[1]

    def op(self, ename, fn, reads=(), writes=(), inc=True):
        e = self.E[ename]
        if e.n >= SEM_ROLL and not e.pending:
            e.gen += 1
            e.sem = self.new_sem(f"p_{e.name}_{e.gen}")
            e.n = 0
        deps = self._collect(e, reads, writes)
        if inc:
            e.n += 1
            e.pending = False
            d = (e.sem, e.n)
        else:
            e.pending = True
            d = (e.sem, e.n + 1)
        incsem = e.sem if inc else None
        sems = self.sems

        def thunk(eng):
            for (k, v) in deps[1:]:
                eng.wait_ge(sems[k], v)
            ins = fn(eng)
            if deps:
                ins._wait_ge(sems[deps[0][0]], deps[0][1])
            if incsem is not None:
                ins.then_inc(sems[incsem], 1)
        e.prog.append(thunk)
        self._mark(d, reads, writes)

    def dma(self, ename, out, in_, reads=(), writes=(), group=None, **kw):
        e = self.E[ename]
        deps = self._collect(e, reads, writes)
        g = group if group is not None else (writes[0] if writes else reads[0])
        if g.name not in self.dma_sem:
            self.dma_sem[g.name] = [self.new_sem(f"d_{g.name}"), 0]
        rec = self.dma_sem[g.name]
        if rec[1] >= SEM_ROLL:
            self.dma_roll = getattr(self, "dma_roll", 0) + 1
            rec[0] = self.new_sem(f"d_{g.name}_{self.dma_roll}")
            rec[1] = 0
        rec[1] += 16
        d = (rec[0], rec[1])
        sems = self.sems
        sk = rec[0]

        def thunk(eng):
            for (k, v) in deps:
                eng.wait_ge(sems[k], v)
            eng.dma_start(out=out, in_=in_, **kw).then_inc(sems[sk], 16)
        e.prog.append(thunk)
        self._mark(d, reads, writes)

    def wait_all(self, ename, bufs):
        e = self.E[ename]
        deps = self._collect(e, bufs, ())
        sems = self.sems

        def thunk(eng):
            for (k, v) in deps:
                eng.wait_ge(sems[k], v)
        e.prog.append(thunk)

    def emit(self):
        with self.nc.Block() as block:
            for name, e in self.E.items():
                def body(eng, e=e):
                    for t in e.prog:
                        t(eng)
                getattr(block, name)(body)


class StopBuild(Exception):
    pass


class K:
    def chk(self, n):
        if self.stage == n:
            raise StopBuild()

    def __init__(self, n_seq, dbg=None, stage=99):
        self.n_seq = n_seq
        self.stage = stage
        self.dbg = dbg or {}
        self.nc = bass.Bass("TRN2", target_bir_lowering=False)
        self.act_rr = 0

    def mm(self, out, lhsT, rhs, start, stop, R, W, inc=None):
        if inc is None:
            inc = stop
        self.fw.op("tensor", lambda t: t.matmul(out, lhsT, rhs, start=start, stop=stop), reads=R, writes=W, inc=inc)

    def tr(self, out, in_, R, W, inc=True):
        idn = self.ident
        self.fw.op("tensor", lambda t: t.transpose(out, in_, idn), reads=R + [self.b_const], writes=W, inc=inc)

    def act(self, out, in_, func, R, W, bias=0.0, scale=1.0, accum=None):
        if accum is None:
            self.fw.op("scalar", lambda s: s.activation(out, in_, func, bias=bias, scale=scale), reads=R, writes=W)
        else:
            self.fw.op("scalar", lambda s: s.activation(out, in_, func, bias=bias, scale=scale, accum_out=accum), reads=R, writes=W)

    def vcopy(self, eng, out, in_, R, W):
        if eng == "scalar":
            self.fw.op("scalar", lambda s: s.copy(out, in_), reads=R, writes=W)
        else:
            self.fw.op(eng, lambda v: v.tensor_copy(out, in_), reads=R, writes=W)

    def tt(self, out, in0, in1, op, R, W, eng="vector"):
        self.fw.op(eng, lambda v: v.tensor_tensor(out, in0, in1, op), reads=R, writes=W)

    def ts(self, out, in0, s1, s2, op0, op1, R, W, accum=None, eng="vector"):
        if op1 is None:
            self.fw.op(eng, lambda v: v.tensor_scalar(out, in0, s1, None, op0), reads=R, writes=W)
        elif accum is None:
            self.fw.op(eng, lambda v: v.tensor_scalar(out, in0, s1, s2, op0, op1), reads=R, writes=W)
        else:
            self.fw.op(eng, lambda v: v.tensor_scalar(out, in0, s1, s2, op0, op1, accum_out=accum), reads=R, writes=W)

    def stt(self, out, in0, scalar, in1, op0, op1, R, W, accum=None):
        if accum is None:
            self.fw.op("vector", lambda v: v.scalar_tensor_tensor(out, in0, scalar, in1, op0, op1), reads=R, writes=W)
        else:
            self.fw.op("vector", lambda v: v.scalar_tensor_tensor(out, in0, scalar, in1, op0, op1, accum_out=accum), reads=R, writes=W)

    def recip(self, out, in_, R, W):
        self.fw.op("vector", lambda v: v.reciprocal(out, in_), reads=R, writes=W)

    def dram(self, name, shape, dt, kind):
        return self.nc.dram_tensor(name, shape, dt, kind=kind).ap()

    def build(self):
        nc = self.nc
        n_seq = self.n_seq
        st = self.stack = ExitStack()
        fw = self.fw = FW(nc, st)
        for n, e in [("sync", nc.sync), ("gpsimd", nc.gpsimd), ("scalar", nc.scalar), ("vector", nc.vector), ("tensor", nc.tensor)]:
            fw.add_engine(n, e)

        x = self.dram("x", [n_seq, S, D], F32, "ExternalInput")
        pos = self.dram("pos", [n_seq, S], I32, "ExternalInput")
        wspec = [("w_in", 1024, DIN), ("w_krsw", 1024, 96), ("w_ki4", 1024, 128), ("w_uq", 256, 768), ("w_uqsw", 256, 768),
                 ("w_ukvk", 256, 512), ("w_ukvv", 256, 512), ("w_a", 512, 1024), ("w_b", 512, 1024), ("w_out", 1024, 1024),
                 ("f_in", 1024, 2 * DFF), ("f_down", DFF, 1024)]
        wf, wb = {}, {}
        for nm, r, c in wspec:
            wf[nm] = self.dram(nm, [r, c], F32, "ExternalInput")
            wb[nm] = self.dram(nm + "_bf", [r, c], BF16, "Internal")
        gq_d = self.dram("gq", [128, 2], F32, "ExternalInput")
        gkv_d = self.dram("gkv", [128, 2], F32, "ExternalInput")
        lnp_d = self.dram("lnp", [4, D], F32, "ExternalInput")
        lnT_d = self.dram("lnT", [128, 16], F32, "ExternalInput")
        cst_d = self.dram("cst", [128, 128 * 3 + NIT + 3], F32, "ExternalInput")
        out = self.dram("out", [n_seq, S, D], F32, "ExternalOutput")
        dbg_out = {}
        for nm, shp in self.dbg.items():
            dbg_out[nm] = self.dram("dbg_" + nm, list(shp), F32, "ExternalOutput")
        self.dbg_out = dbg_out

        def sb(name, shape, dt):
            return st.enter_context(nc.sbuf_tensor("s_" + name, shape, dt))

        cstf = sb("cstf", [128, 128 * 3 + NIT + 3], F32)
        identb = sb("identb", [128, 128], BF16)
        tri01 = sb("tri01", [128, 128], BF16)
        onesb = sb("onesb", [128, 128], BF16)
        gq = sb("gq", [128, 2], F32)
        gkv = sb("gkv", [128, 2], F32)
        lnT = sb("lnT", [128, 16], F32)
        self.ident = identb[:]
        negtri = cstf[:, 256:384]
        pow2 = cstf[:, 384:384 + NIT + 1]
        freq = cstf[:, 384 + NIT + 1:384 + NIT + 2]
        sgn = cstf[:, 384 + NIT + 2:384 + NIT + 3]
        b_const = self.b_const = Buf("const")
        fw.dma("gpsimd", cstf[:], cst_d[:], writes=[b_const])
        fw.dma("gpsimd", gq[:], gq_d[:], writes=[b_const])
        fw.dma("gpsimd", gkv[:], gkv_d[:], writes=[b_const])
        fw.dma("gpsimd", lnT[:], lnT_d[:], writes=[b_const])
        self.vcopy("vector", identb[:], cstf[:, 0:128], [b_const], [b_const])
        self.vcopy("vector", tri01[:], cstf[:, 128:256], [b_const], [b_const])
        fw.op("vector", lambda v: v.memset(onesb[:], 1.0), writes=[b_const])

        b_w = {}
        dims = {nm: (r, c) for nm, r, c in wspec}
        for nm in dims:
            b_w[nm] = Buf("wscr_" + nm)

        def prepass(names):
            for nm in names:
                r, c = dims[nm]
                rows = (r * c) // 1024
                src = wf[nm].rearrange("r c -> (r c)").rearrange("(a b) -> a b", b=1024)
                dst = wb[nm].rearrange("r c -> (r c)").rearrange("(a b) -> a b", b=1024)
                a0 = 0
                while a0 < rows:
                    a1 = min(rows, a0 + 2048)
                    fw.dma("gpsimd", dst[a0:a1, :], src[a0:a1, :], writes=[Buf("wscr_tmp")], group=b_w[nm])
                    a0 = a1
                b_w[nm].w = tuple(fw.dma_sem[b_w[nm].name])
        prepass(["w_krsw", "w_uq", "w_uqsw", "w_ukvk", "w_ukvv", "w_ki4", "w_in"])
        self.b_w = b_w

        P_ELEMS = 28 * 1024
        Pa = sb("arenaP", [128, P_ELEMS], BF16)
        o_aT = sb("o_aT", [128, 4, S], BF16)
        T_ELEMS = 38 * 1024
        Ta = sb("arenaT", [128, T_ELEMS], BF16)
        NSLAB = 4
        slabs = [sb(f"slab{i}", [128, SLABW], BF16) for i in range(NSLAB)]
        b_slab = [Buf(f"slab{i}") for i in range(NSLAB)]
        self.slab_rr = 0
        small = sb("small", [128, 512], F32)
        b_small = Buf("small")

        banks = [st.enter_context(nc.psum_tensor(f"bank{i}", [128, 512], F32)) for i in range(8)]
        b_bank = [Buf(f"bank{i}") for i in range(8)]

        V_OFF = 16 * 1024
        Vt = Pa[:, V_OFF:V_OFF + 16 * 768].rearrange("p (c q e) -> p c q e", c=16, q=4)
        b_V = [Buf(f"V{g}") for g in range(NG)]
        b_V1 = Buf("Vones")
        fw.op("vector", lambda v: v.memset(Vt[:, :, :, 64:128], 1.0), writes=[b_V1])
        KA = [Pa[:, h * 2048:(h + 1) * 2048] for h in range(8)]
        b_KA = [[Buf(f"KA{h}_{g}") for g in range(NG)] for h in range(8)]
        KB = [Pa[:, p * 2048:(p + 1) * 2048] for p in range(4)]
        kiT = Pa[:, 4 * 2048:5 * 2048]
        b_KB = [b_KA[p] for p in range(4)]
        b_ki = b_KA[4]
        b_oa = [Buf(f"oa{g}") for g in range(NG)]

        RB = [Buf(f"T{i}") for i in range(76)]

        def tv(kb0, kb1, dt, shape=None):
            ap = Ta[:, kb0 * 512:kb1 * 512]
            if dt != BF16:
                ap = ap.bitcast(dt)
            return ap, RB[kb0:kb1]

        def slab_load(parts):
            i = self.slab_rr % NSLAB
            self.slab_rr += 1
            views = []
            for (off, kc, ncol, src) in parts:
                v = slabs[i][:, off:off + kc * ncol].rearrange("p (k n) -> p k n", k=kc)
                wname = src.tensor.name[:-3]
                fw.dma("sync", v, src.rearrange("(k p) n -> p k n", p=128), reads=[b_w[wname]], writes=[b_slab[i]])
                views.append(v)
            return views, b_slab[i]

        evac_rr = [0]

        def evac_eng():
            evac_rr[0] += 1
            return "scalar" if evac_rr[0] % 2 else "vector"

        b_outs = []

        def dbg_store(name, ap, bufs):
            if name in dbg_out:
                b = Buf("dbg_" + name)
                if ap.dtype != F32:
                    fw.dma("gpsimd", dbg_out[name][:], ap, reads=bufs, writes=[b], max_dma_last_dim=1024)
                else:
                    fw.dma("gpsimd", dbg_out[name][:], ap, reads=bufs, writes=[b])
                b_outs.append(b)

        xissued = set()
        xseq = []
        for sq_ in range(n_seq):
            for ps_ in ("A", "B"):
                for g_ in range(NG):
                    for j_ in range(4):
                        xseq.append((ps_, sq_, g_, j_))
        xpos = {k: i for i, k in enumerate(xseq)}

        def issue_x(idx):
            if idx >= len(xseq) or idx in xissued:
                return
            xissued.add(idx)
            ps_, sq_, g_, j_ = xseq[idx]
            xb, b_xb = tv(62 + 2 * (idx % 3), 64 + 2 * (idx % 3), BF16)
            t0 = g_ * GT + j_ * 128
            fw.dma("gpsimd", xb, x[sq_, t0:t0 + 128, :], writes=b_xb)

        def load_x_group(sq, g, ps):
            xT, b_xT = tv(32, 40, BF16)
            xT = xT.rearrange("p (k n) -> p k n", k=8)
            base = xpos[(ps, sq, g, 0)]
            for j in range(4):
                idx = base + j
                issue_x(idx)
                if j < 3:
                    issue_x(idx + 1)
                xb, b_xb = tv(62 + 2 * (idx % 3), 64 + 2 * (idx % 3), BF16)
                for half in range(2):
                    bk = 6 + half
                    pv = banks[bk][:].bitcast(BF16)
                    for c4 in range(4):
                        c = half * 4 + c4
                        self.tr(pv[:, c4 * 128:(c4 + 1) * 128], xb[:, c * 128:(c + 1) * 128], b_xb, [b_bank[bk]], inc=(c4 == 3))
                    src = pv[:, 0:512].rearrange("p (c n) -> p c n", c=4)
                    self.vcopy(evac_eng(), xT[:, half * 4:(half + 1) * 4, j * 128:(j + 1) * 128], src, [b_bank[bk]], b_xT)
            if ps == "A":
                for d_ in range(3):
                    issue_x(base + 4 + d_)
            return xT, b_xT

        def rope_tables(sq, g):
            posi, b_posi = tv(40, 42, I32)
            ang, b_ang = tv(42, 44, F32)
            tmp, b_tmp = tv(44, 46, F32)
            kk, b_kk = tv(46, 48, I32)
            cos, b_cos = tv(48, 50, F32)
            sin, b_sin = tv(50, 52, F32)
            r = slice(64, 96)
            fw.dma("gpsimd", posi[r, :], pos[sq, g * GT:(g + 1) * GT].partition_broadcast(32), writes=b_posi)
            self.vcopy("vector", tmp[r, :], posi[r, :], b_posi, b_tmp)
            self.ts(ang[r, :], tmp[r, :], freq[r, :], None, ALU.mult, None, b_tmp + [b_const], b_ang)
            for which, dst, b_dst, shift in (("sin", sin, b_sin, 0.0), ("cos", cos, b_cos, math.pi / 2)):
                self.ts(tmp[r, :], ang[r, :], shift, 1.0 / (2 * math.pi), ALU.add, ALU.mult, b_ang, b_tmp)
                self.vcopy("vector", kk[r, :], tmp[r, :], b_tmp, b_kk)
                self.vcopy("vector", tmp[r, :], kk[r, :], b_kk, b_tmp)
                self.ts(tmp[r, :], tmp[r, :], -2 * math.pi, shift, ALU.mult, ALU.add, b_tmp, b_tmp)
                self.tt(dst[r, :], tmp[r, :], ang[r, :], ALU.add, b_tmp + b_ang, b_dst)
                self.ts(tmp[r, :], dst[r, :], math.pi, -2 * math.pi, ALU.is_gt, ALU.mult, b_dst, b_tmp)
                self.tt(dst[r, :], dst[r, :], tmp[r, :], ALU.add, b_dst + b_tmp, b_dst)
                self.ts(tmp[r, :], dst[r, :], -math.pi, 2 * math.pi, ALU.is_lt, ALU.mult, b_dst, b_tmp)
                self.tt(dst[r, :], dst[r, :], tmp[r, :], ALU.add, b_dst + b_tmp, b_dst)
                self.ts(dst[r, :], dst[r, :], math.pi, -math.pi, ALU.min, ALU.max, b_dst, b_dst)
                if which == "sin":
                    self.act(dst[r, :], dst[r, :], AF.Sin, b_dst + [b_const], b_dst, scale=sgn[r, :])
                else:
                    self.act(dst[r, :], dst[r, :], AF.Sin, b_dst, b_dst)
            return cos, b_cos, sin, b_sin

        def rms_norm_fm(ps_list, gvec, outT, b_outT, sq_t, b_sq, rs, b_rs):
            for rr, bk in enumerate(ps_list):
                self.act(sq_t[:, rr, :], banks[bk][:], AF.Square, [b_bank[bk]], b_sq)
            for rr in range(2):
                self.mm(banks[5][:], onesb[:], sq_t[:, rr, :], rr == 0, rr == 1, b_sq + [b_const], [b_bank[5]])
            self.ts(rs, banks[5][:], 1.0 / 256.0, RMS_EPS, ALU.mult, ALU.add, [b_bank[5]], b_rs)
            self.act(rs, rs, AF.Sqrt, b_rs, b_rs)
            self.recip(rs, rs, b_rs, b_rs)
            for rr, bk in enumerate(ps_list):
                self.stt(outT[:, rr, :], banks[bk][:], gvec[:, rr:rr + 1], rs, ALU.mult, ALU.mult, [b_bank[bk], b_const] + b_rs, b_outT)

        def attention(g, nheads, kfn, qfn, vsel, scale, oT, b_oT, bias_fn=None, mask=None):
            NPT, NS, LA = 8, 4, 3
            PT = [tv(68 + i, 69 + i, BF16) for i in range(NPT)]
            SBK = [2, 3, 6, 7]
            rd, b_rd = tv(56, 58, F32)
            nch = 4 * g + 4
            items = [(h, c) for h in range(nheads) for c in range(nch)]

            def emit_score(idx):
                h, c = items[idx]
                n0 = 0 if c < 4 * g else (c - 4 * g) * 128
                sbk = SBK[idx % NS]
                pt, b_pt = PT[idx % NPT]
                kap, kb = kfn(h, c)
                qap, qb = qfn(h, n0)
                self.mm(banks[sbk][:, n0:512], kap, qap, True, True, kb + qb, [b_bank[sbk]])
                if bias_fn is None:
                    self.act(pt[:, n0:512], banks[sbk][:, n0:512], AF.Exp, [b_bank[sbk]], b_pt, scale=scale)
                else:
                    for (b0, b1, bias_ap, bb) in bias_fn(h, c, n0):
                        self.act(pt[:, b0:b1], banks[sbk][:, b0:b1], AF.Exp, [b_bank[sbk]] + bb, b_pt, bias=bias_ap, scale=scale)
                if mask is not None:
                    map_, mb = mask(c, n0)
                    self.tt(pt[:, n0:512], pt[:, n0:512], map_, ALU.mult, b_pt + mb, b_pt)
                elif c >= 4 * g:
                    self.tt(pt[:, n0:n0 + 128], pt[:, n0:n0 + 128], tri01[:], ALU.mult, b_pt + [b_const], b_pt)

            def emit_pv(idx):
                h, c = items[idx]
                n0 = 0 if c < 4 * g else (c - 4 * g) * 128
                acc = 4 + (h % 2)
                pt, b_pt = PT[idx % NPT]
                vap, vb = vsel(h, c)
                self.mm(banks[acc][:, n0:512], vap, pt[:, n0:512], c == 0, c == nch - 1, vb + b_pt, [b_bank[acc]], inc=(c == nch - 1))
                if c == nch - 1:
                    pr = h // 2
                    if h % 2 == 0:
                        self.recip(rd[0:64, :], banks[acc][64:128, :], [b_bank[acc]], b_rd)
                        self.tt(oT[0:64, pr, :], banks[acc][0:64, :], rd[0:64, :], ALU.mult, [b_bank[acc]] + b_rd, b_oT)
                    else:
                        self.recip(rd[64:128, :], banks[acc][0:64, :], [b_bank[acc]], b_rd)
                        self.tt(oT[64:128, pr, :], banks[acc][64:128, :], rd[64:128, :], ALU.mult, [b_bank[acc]] + b_rd, b_oT)

            for idx in range(len(items) + LA):
                if idx < len(items):
                    emit_score(idx)
                if idx - LA >= 0:
                    emit_pv(idx - LA)

        def pass_a_group(sq, g):
            gs = slice(g * GT, (g + 1) * GT)
            xT, b_xT = load_x_group(sq, g, "A")
            dbg_store("xT", xT.rearrange("p k n -> p (k n)"), b_xT)
            self.chk(1)
            (w1,), bw1 = slab_load([(0, 8, 544, wb["w_in"][:, 0:544])])
            (wkrsw, wuq, wuqsw), bw2 = slab_load([(0, 8, 96, wb["w_krsw"][:, :]), (768, 2, 768, wb["w_uq"][:, :]), (2304, 2, 768, wb["w_uqsw"][:, :])])
            (wukvk, wukvv), bw3 = slab_load([(0, 2, 512, wb["w_ukvk"][:, :]), (1024, 2, 512, wb["w_ukvv"][:, :])])
            cos, b_cos, sin, b_sin = rope_tables(sq, g)
            cqT, b_cq = tv(0, 2, BF16)
            cqT = cqT.rearrange("p (r n) -> p r n", r=2)
            ckvT, b_ckv = tv(2, 4, BF16)
            ckvT = ckvT.rearrange("p (r n) -> p r n", r=2)
            sq_t, b_sq = tv(4, 6, BF16)
            sq_t = sq_t.rearrange("p (r n) -> p r n", r=2)
            rs, b_rs = tv(6, 8, F32)
            t1, b_t1 = tv(8, 10, F32)
            t2, b_t2 = tv(10, 12, F32)
            for which, col0, outT, b_o, gvec in (("q", C_CQ, cqT, b_cq, gq), ("kv", C_CKV, ckvT, b_ckv, gkv)):
                for rr in range(2):
                    for k in range(8):
                        self.mm(banks[rr][:], w1[:, k, col0 + rr * 128:col0 + (rr + 1) * 128], xT[:, k, :], k == 0, k == 7, [bw1] + b_xT, [b_bank[rr]])
                rms_norm_fm([0, 1], gvec, outT, b_o, sq_t, b_sq, rs, b_rs)
            for k in range(8):
                self.mm(banks[0][0:96, :], w1[:, k, 448:544], xT[:, k, :], k == 0, k == 7, [bw1] + b_xT, [b_bank[0]])
            for k in range(8):
                self.mm(banks[1][0:96, :], wkrsw[:, k, :], xT[:, k, :], k == 0, k == 7, [bw2] + b_xT, [b_bank[1]])
            r = slice(64, 96)
            self.tt(t1[r, :], banks[0][r, :], cos[r, :], ALU.mult, [b_bank[0]] + b_cos, b_t1)
            self.tt(t2[r, :], banks[1][r, :], sin[r, :], ALU.mult, [b_bank[1]] + b_sin, b_t2)
            for h in range(8):
                self.tt(KA[h][r, gs], t1[r, :], t2[r, :], ALU.add, b_t1 + b_t2, [b_KA[h][g]])
            for pr in range(4):
                bk = pr
                for rr in range(2):
                    self.mm(banks[bk][:], wukvk[:, rr, pr * 128:(pr + 1) * 128], ckvT[:, rr, :], rr == 0, rr == 1, [bw3] + b_ckv, [b_bank[bk]])
                self.vcopy("scalar", KA[2 * pr][0:64, gs], banks[bk][0:64, :], [b_bank[bk]], [b_KA[2 * pr][g]])
                self.vcopy("vector", KA[2 * pr + 1][0:64, gs], banks[bk][64:128, :], [b_bank[bk]], [b_KA[2 * pr + 1][g]])
            for j in range(4):
                bk = j
                for rr in range(2):
                    self.mm(banks[bk][:], ckvT[:, rr, j * 128:(j + 1) * 128], wukvv[:, rr, :], rr == 0, rr == 1, [bw3] + b_ckv, [b_bank[bk]])
                src = banks[bk][:].rearrange("p (q e d) -> p q e d", q=4, e=2)
                self.vcopy("scalar", Vt[:, g * 4 + j, :, 0:64], src[:, :, 0, :], [b_bank[bk]], [b_V[g]])
                self.vcopy("vector", Vt[:, g * 4 + j, :, 128:192], src[:, :, 1, :], [b_bank[bk]], [b_V[g]])
            dbg_store("cos", cos[64:96, :], b_cos)
            dbg_store("sin", sin[64:96, :], b_sin)
            self.chk(2)
            QA, b_QA = tv(12, 20, BF16)
            QA = QA.rearrange("p (h n) -> p h n", h=8)
            b_QAh = [b_QA[h:h + 1] for h in range(8)]
            for h in range(8):
                ba, bb_ = (0, 1) if h % 2 == 0 else (4, 5)
                for rr in range(2):
                    self.mm(banks[ba][0:96, :], wuq[:, rr, h * 96:(h + 1) * 96], cqT[:, rr, :], rr == 0, rr == 1, [bw2] + b_cq, [b_bank[ba]])
                for rr in range(2):
                    self.mm(banks[bb_][0:96, :], wuqsw[:, rr, h * 96:(h + 1) * 96], cqT[:, rr, :], rr == 0, rr == 1, [bw2] + b_cq, [b_bank[bb_]])
                self.vcopy("vector", QA[0:64, h, :], banks[ba][0:64, :], [b_bank[ba]], b_QAh[h])
                self.tt(t1[r, :], banks[ba][r, :], cos[r, :], ALU.mult, [b_bank[ba]] + b_cos, b_t1)
                self.tt(t2[r, :], banks[bb_][r, :], sin[r, :], ALU.mult, [b_bank[bb_]] + b_sin, b_t2)
                self.tt(QA[r, h, :], t1[r, :], t2[r, :], ALU.add, b_t1 + b_t2, b_QAh[h])
            if g == 1:
                dbg_store("QA0", QA[0:96, 0, :], b_QA)
                dbg_store("KA0", KA[0][0:96, 0:1024], [b_KA[0][0], b_KA[0][1]])
                dbg_store("cqT", cqT.rearrange("p r n -> p (r n)"), b_cq)

            self.chk(3)

            def kfn(h, c):
                return KA[h][0:96, c * 128:(c + 1) * 128], [b_KA[h][c // 4]]

            def qfn(h, n0):
                return QA[0:96, h, n0:512], b_QAh[h]

            def vsel(h, c):
                pr = h // 2
                if h % 2 == 0:
                    return Vt[:, c, pr, 0:128], [b_V[c // 4], b_V1]
                return Vt[:, c, pr, 64:192], [b_V[c // 4], b_V1]
            attention(g, 8, kfn, qfn, vsel, SC_MLA, o_aT[:, :, gs], [b_oa[g]])

        def pass_b_group(sq, g):
            gs = slice(g * GT, (g + 1) * GT)
            xT, b_xT = load_x_group(sq, g, "B")
            (wqb,), bwq = slab_load([(0, 8, 512, wb["w_in"][:, C_QB:C_QB + 512])])
            (wkb,), bwk = slab_load([(0, 8, 512, wb["w_in"][:, C_KB:C_KB + 512])])
            (wvb,), bwv = slab_load([(0, 8, 512, wb["w_in"][:, C_VB:C_VB + 512])])
            (wqi, wki, wwi), bwi = slab_load([(0, 8, 256, wb["w_in"][:, C_QI:C_QI + 256]), (2048, 8, 128, wb["w_ki4"][:, :]),
                                              (3072, 8, 8, wb["w_in"][:, C_WI:C_WI + 8])])
            QB, b_QB = tv(0, 4, BF16)
            QB = QB.rearrange("p (q n) -> p q n", q=4)
            qiT, b_qi = tv(4, 6, BF16)
            qiT = qiT.rearrange("p (q n) -> p q n", q=2)
            for pr in range(4):
                bk = pr
                for k in range(8):
                    self.mm(banks[bk][:], wqb[:, k, pr * 128:(pr + 1) * 128], xT[:, k, :], k == 0, k == 7, [bwq] + b_xT, [b_bank[bk]])
                self.vcopy(evac_eng(), QB[:, pr, :], banks[bk][:], [b_bank[bk]], b_QB)
            for pr in range(4):
                bk = pr
                for k in range(8):
                    self.mm(banks[bk][:], wkb[:, k, pr * 128:(pr + 1) * 128], xT[:, k, :], k == 0, k == 7, [bwk] + b_xT, [b_bank[bk]])
                self.vcopy(evac_eng(), KB[pr][:, gs], banks[bk][:], [b_bank[bk]], [b_KB[pr][g]])
            for j in range(4):
                bk = j
                for k in range(8):
                    self.mm(banks[bk][:], xT[:, k, j * 128:(j + 1) * 128], wvb[:, k, :], k == 0, k == 7, [bwv] + b_xT, [b_bank[bk]])
                src = banks[bk][:].rearrange("p (q e d) -> p q e d", q=4, e=2)
                self.vcopy("scalar", Vt[:, g * 4 + j, :, 0:64], src[:, :, 0, :], [b_bank[bk]], [b_V[g]])
                self.vcopy("vector", Vt[:, g * 4 + j, :, 128:192], src[:, :, 1, :], [b_bank[bk]], [b_V[g]])
            for q2 in range(2):
                bk = q2
                for k in range(8):
                    self.mm(banks[bk][:], wqi[:, k, q2 * 128:(q2 + 1) * 128], xT[:, k, :], k == 0, k == 7, [bwi] + b_xT, [b_bank[bk]])
                self.vcopy(evac_eng(), qiT[:, q2, :], banks[bk][:], [b_bank[bk]], b_qi)
            for k in range(8):
                self.mm(banks[0][:], wki[:, k, :], xT[:, k, :], k == 0, k == 7, [bwi] + b_xT, [b_bank[0]])
            self.vcopy(evac_eng(), kiT[:, gs], banks[0][:], [b_bank[0]], [b_ki[g]])
            wI = small[:, 0:32].rearrange("p (j h) -> p j h", j=4)
            for j in range(4):
                for k in range(8):
                    self.mm(banks[1][:, j * 8:(j + 1) * 8], xT[:, k, j * 128:(j + 1) * 128], wwi[:, k, :], k == 0, k == 7, [bwi] + b_xT, [b_bank[1]])
            self.vcopy("vector", small[:, 0:32], banks[1][:, 0:32], [b_bank[1]], [b_small])
            self.chk(6)
            posT_i = small[:, 64:80].bitcast(I32)
            posT = small[:, 80:96]
            posq_i = small[:, 96:100].bitcast(I32)
            posq = small[:, 100:104]
            fw.dma("gpsimd", posT_i, pos[sq, :].rearrange("(c p) -> p c", p=128), writes=[b_small], allow_slow_non_contiguous=True)
            fw.dma("gpsimd", posq_i, pos[sq, g * GT:(g + 1) * GT].rearrange("(j p) -> j p", p=128)[:, 0].partition_broadcast(128), writes=[b_small], allow_slow_non_contiguous=True)
            self.vcopy("vector", posT, posT_i, [b_small], [b_small])
            self.vcopy("vector", posq, posq_i, [b_small], [b_small])
            biasall = small[:, 128:192].rearrange("p (c j) -> p c j", c=16)
            self.tt(biasall, posT.unsqueeze(2).to_broadcast([128, 16, 4]), posq.unsqueeze(1).to_broadcast([128, 16, 4]), ALU.subtract, [b_small], [b_small])
            biash = [small[:, 192 + 0:192 + 64], small[:, 256:320]]
            b_biash = [Buf("biash0"), Buf("biash1")]

            R, b_R = tv(6, 14, BF16)
            R = R.rearrange("p (h n) -> p h n", h=8)
            scsL = [tv(14, 22, F32), tv(68, 76, F32)]
            mskL = [tv(22, 26, BF16), tv(6, 10, BF16)]
            maskT, b_maskT = tv(40, 56, BF16)
            maskT = maskT.rearrange("p (c n) -> p c n", c=16)
            Dw, b_Dw = tv(26, 28, BF16)
            Dw = Dw.rearrange("p (h n) -> p h n", h=8)
            b_ch = [Buf(f"chain{q}_{sq}_{g}") for q in range(2)]
            chs = []
            for q in range(2):
                bis = small[:, 320 + 16 * q:336 + 16 * q]
                chs.append(dict(A=bis[:, 0:1], W=bis[:, 1:2], mid=bis[:, 2:3], cnt=bis[:, 3:4], gs=bis[:, 4:5], thr=bis[:, 5:6], mid2=bis[:, 6:7],
                                steps=small[:, 352 + q * (NIT + 1):352 + (q + 1) * (NIT + 1)], b=[b_ch[q]]))
            for jp in (0, 2):
                info = []
                for q in range(2):
                    j = jp + q
                    qb = 4 * g + j
                    svis = (qb + 1) * 128
                    scs, b_scs = scsL[q]
                    ch = chs[q]
                    for h in range(8):
                        self.ts(Dw[:, h, :], identb[:], wI[:, j, h:h + 1], None, ALU.mult, None, [b_const, b_small], b_Dw)
                    nsg = (svis + 511) // 512
                    for sg in range(nsg):
                        ncol = min(512, svis - sg * 512)
                        scb = 2 if sg % 2 == 0 else 5
                        for h in range(8):
                            bk = (0, 1, 3, 4)[h % 4]
                            rows = slice((h % 4) * 32, (h % 4) * 32 + 32)
                            tp = ((h % 4) * 32, 0)
                            lhs = qiT[rows, h // 4, j * 128:(j + 1) * 128]
                            rhs = kiT[rows, sg * 512:sg * 512 + ncol]
                            fw.op("tensor", (lambda o, l, r_, tp_: (lambda t: t.matmul(o, l, r_, start=True, stop=True, tile_position=tp_)))(banks[bk][:, 0:ncol], lhs, rhs, tp),
                                  reads=b_qi + [b_ki[sg]], writes=[b_bank[bk]])
                            if h % 2:
                                self.act(R[:, h, 0:ncol], banks[bk][:, 0:ncol], AF.Relu, [b_bank[bk]], b_R[h:h + 1])
                            else:
                                self.ts(R[:, h, 0:ncol], banks[bk][:, 0:ncol], 0.0, None, ALU.max, None, [b_bank[bk]], b_R[h:h + 1])
                        for h in range(8):
                            self.mm(banks[scb][:, 0:ncol], Dw[:, h, :], R[:, h, 0:ncol], h == 0, h == 7, b_Dw + b_R[h:h + 1], [b_bank[scb]])
                        self.vcopy("scalar", scs[:, sg * 512:sg * 512 + ncol], banks[scb][:, 0:ncol], [b_bank[scb]], b_scs)
                    dcol = slice(qb * 128, (qb + 1) * 128)
                    if qb >= 2:
                        fw.op("vector", lambda v, o=ch["A"], i=scs[:, 0:svis]: v.tensor_reduce(o, i, AX.X, ALU.max, apply_absolute_value=True), reads=b_scs, writes=ch["b"])
                    self.tt(scs[:, dcol], scs[:, dcol], negtri, ALU.add, b_scs + [b_const], b_scs)
                    info.append((j, qb, svis))
                if info[0][1] >= 2:
                    for q in range(2):
                        ch = chs[q]
                        sgn_ = 1.0 if q == 0 else -1.0
                        self.ts(ch["W"], ch["A"], 2.0 * sgn_, 1e-3 * sgn_, ALU.mult, ALU.add, ch["b"], ch["b"])
                        self.ts(ch["steps"], pow2, ch["W"], None, ALU.mult, None, ch["b"] + [b_const], ch["b"])
                        self.stt(ch["mid"], ch["A"], -1.0 * sgn_, ch["steps"][:, 0:1], ALU.mult, ALU.add, ch["b"], ch["b"])
                        self.ts(ch["mid"], ch["mid"], -5e-4 * sgn_, None, ALU.add, None, ch["b"], ch["b"])
                    for i in range(NIT):
                        ch = chs[0]
                        j, qb, svis = info[0]
                        scs, b_scs = scsL[0]
                        jk, b_jk = mskL[0]
                        self.ts(jk[:, 0:svis], scs[:, 0:svis], ch["mid"], None, ALU.is_ge, ALU.add, b_scs + ch["b"], b_jk + ch["b"], accum=ch["cnt"])
                        self.ts(ch["gs"], ch["cnt"], TOPK - 0.5, ch["steps"][:, i:i + 1], ALU.is_ge, ALU.mult, ch["b"], ch["b"])
                        if i < NIT - 1:
                            self.stt(ch["mid"], ch["mid"], ch["steps"][:, i + 1:i + 2], ch["gs"], ALU.subtract, ALU.add, ch["b"], ch["b"])
                        else:
                            self.stt(ch["thr"], ch["mid"], ch["steps"][:, i:i + 1], ch["gs"], ALU.subtract, ALU.add, ch["b"], ch["b"])
                        ch = chs[1]
                        j, qb, svis = info[1]
                        scs, b_scs = scsL[1]
                        jk, b_jk = mskL[1]
                        cur = ch["mid"] if i % 2 == 0 else ch["mid2"]
                        nxt = ch["mid2"] if i % 2 == 0 else ch["mid"]
                        self.act(jk[:, 0:svis], scs[:, 0:svis], AF.Sign, b_scs + ch["b"], b_jk + ch["b"], bias=cur, scale=1.0, accum=ch["cnt"])
                        self.act(ch["gs"], ch["cnt"], AF.Sign, ch["b"], ch["b"], bias=-(2.0 * TOPK - svis - 0.5), scale=1.0)
                        self.act(nxt, ch["gs"], AF.Identity, ch["b"], ch["b"], bias=cur, scale=ch["steps"][:, i + 1:i + 2])
                    ch = chs[1]
                    fin = ch["mid"] if NIT % 2 == 0 else ch["mid2"]
                    self.ts(ch["thr"], fin, -1.0, ch["steps"][:, NIT:NIT + 1], ALU.mult, ALU.add, ch["b"], ch["b"])
                for q in range(2):
                    ch = chs[q]
                    j, qb, svis = info[q]
                    scs, b_scs = scsL[q]
                    mask, b_mask = mskL[q]
                    if qb >= 2:
                        self.ts(mask[:, 0:svis], scs[:, 0:svis], ch["thr"], None, ALU.is_ge, None, b_scs + ch["b"], b_mask)
                    else:
                        self.ts(mask[:, 0:svis], scs[:, 0:svis], -1e29, None, ALU.is_ge, None, b_scs, b_mask)
                    c = 0
                    while c <= qb:
                        n = min(8, qb + 1 - c)
                        bk = 6 + ((c // 8) % 2)
                        pv = banks[bk][:].bitcast(BF16)
                        for i in range(n):
                            self.tr(pv[:, i * 128:(i + 1) * 128], mask[:, (c + i) * 128:(c + i + 1) * 128], b_mask, [b_bank[bk]], inc=(i == n - 1))
                        self.vcopy(evac_eng(), maskT[:, c:c + n, j * 128:(j + 1) * 128], pv[:, 0:n * 128].rearrange("p (c n) -> p c n", c=n), [b_bank[bk]], b_maskT)
                        c += n
            if g == 1:
                dbg_store("maskT", maskT[:, 0:8, :].rearrange("p c n -> p (c n)"), b_maskT)

            self.chk(7)
            o_bT, b_ob = tv(58, 62, BF16)
            o_bT = o_bT.rearrange("p (q n) -> p q n", q=4)
            cur = {"h": -1}

            def bias_fn(h, c, n0):
                k = h % 2
                if cur["h"] != h:
                    cur["h"] = h
                    self.ts(biash[k], small[:, 128:192], SLOPES[h], None, ALU.mult, None, [b_small], [b_biash[k]])
                span = 128 if h == 0 else (256 if h == 1 else 512)
                res = []
                b0 = n0
                while b0 < 512:
                    blk_start = (b0 // span) * span
                    b1 = min(512, blk_start + span)
                    jj = blk_start // 128
                    res.append((b0, b1, biash[k].rearrange("p (c j) -> p c j", c=16)[:, c, jj:jj + 1], [b_biash[k]]))
                    b0 = b1
                return res

            def kfn(h, c):
                return KB[h // 2][(h % 2) * 64:(h % 2) * 64 + 64, c * 128:(c + 1) * 128], [b_KB[h // 2][c // 4]]

            def qfn(h, n0):
                return QB[(h % 2) * 64:(h % 2) * 64 + 64, h // 2, n0:512], b_QB

            def vsel(h, c):
                pr = h // 2
                if h % 2 == 0:
                    return Vt[:, c, pr, 0:128], [b_V[c // 4], b_V1]
                return Vt[:, c, pr, 64:192], [b_V[c // 4], b_V1]

            def maskfn(c, n0):
                return maskT[:, c, n0:512], b_maskT
            attention(g, 8, kfn, qfn, vsel, SC_DSA, o_bT, b_ob, bias_fn=bias_fn, mask=maskfn)
            if g == 1:
                dbg_store("obT", o_bT.rearrange("p q n -> p (q n)"), b_ob)
            self.chk(8)
            phase2(sq, g, xT, b_xT, o_bT, b_ob)

        def layer_norm_tile(z, b_z, gsel, outap, b_outap, lng, lnb, b_ln):
            stats = small[:, 400:412].rearrange("p (c s) -> p c s", c=2)
            mv = small[:, 412:414]
            rstd = small[:, 414:415]
            for hh in range(2):
                fw.op("vector", lambda v, o=stats[:, hh, :], i=z[:, hh * 512:(hh + 1) * 512]: v.bn_stats(o, i), reads=b_z, writes=[b_small])
            fw.op("vector", lambda v: v.bn_aggr(mv, small[:, 400:412]), reads=[b_small], writes=[b_small])
            self.ts(rstd, mv[:, 1:2], LN_EPS, None, ALU.add, None, [b_small], [b_small])
            self.act(rstd, rstd, AF.Sqrt, [b_small], [b_small])
            self.recip(rstd, rstd, [b_small], [b_small])
            self.ts(z, z, mv[:, 0:1], rstd, ALU.subtract, ALU.mult, b_z + [b_small], b_z)
            self.tt(z, z, lng, ALU.mult, b_z + b_ln, b_z, eng="gpsimd")
            self.tt(outap, z, lnb, ALU.add, b_z + b_ln, b_outap)

        def phase2(sq, g, xT, b_xT, o_bT, b_ob):
            gs = slice(g * GT, (g + 1) * GT)
            mixedT, b_mx = tv(0, 8, BF16)
            mixedT = mixedT.rearrange("p (k n) -> p k n", k=8)
            lnp, b_lnp = tv(8, 16, F32)
            h1, b_h1 = tv(16, 32, F32)
            h1 = h1.rearrange("p (j n) -> p j n", j=4)
            b_h1j = [b_h1[4 * j:4 * j + 4] for j in range(4)]
            h1T, b_h1T = tv(32, 40, BF16)
            h1T = h1T.rearrange("p (k n) -> p k n", k=8)
            actT, b_act = tv(40, 62, BF16)
            actT = actT.rearrange("p (f n) -> p f n", f=NF)
            xf, b_xf = tv(62, 66, F32)
            h1b, b_h1b = tv(66, 68, BF16)
            sga, b_sga = tv(68, 69, BF16)
            sgb, b_sgb = tv(69, 70, BF16)
            t1, b_t1 = tv(70, 72, F32)
            t2, b_t2 = tv(72, 74, F32)
            for half in range(2):
                (wa,), bwa = slab_load([(0, 4, 1024, wb["w_a"][:, :])])
                (wb_,), bwb = slab_load([(0, 4, 1024, wb["w_b"][:, :])])
                (wga,), bga = slab_load([(0, 8, 512, wb["w_in"][:, C_GA + half * 512:C_GA + (half + 1) * 512])])
                (wgb,), bgb = slab_load([(0, 8, 512, wb["w_in"][:, C_GB + half * 512:C_GB + (half + 1) * 512])])
                for c4 in range(4):
                    c = half * 4 + c4
                    B0 = (c % 2) * 4
                    for k in range(8):
                        self.mm(banks[B0][:], wga[:, k, c4 * 128:(c4 + 1) * 128], xT[:, k, :], k == 0, k == 7, [bga] + b_xT, [b_bank[B0]])
                    for k in range(8):
                        self.mm(banks[B0 + 1][:], wgb[:, k, c4 * 128:(c4 + 1) * 128], xT[:, k, :], k == 0, k == 7, [bgb] + b_xT, [b_bank[B0 + 1]])
                    for p in range(4):
                        self.mm(banks[B0 + 2][:], wa[:, p, c * 128:(c + 1) * 128], o_aT[:, p, gs], p == 0, p == 3, [bwa, b_oa[g]], [b_bank[B0 + 2]])
                    for p in range(4):
                        self.mm(banks[B0 + 3][:], wb_[:, p, c * 128:(c + 1) * 128], o_bT[:, p, :], p == 0, p == 3, [bwb] + b_ob, [b_bank[B0 + 3]])
                    self.act(sga, banks[B0][:], AF.Sigmoid, [b_bank[B0]], b_sga)
                    self.act(sgb, banks[B0 + 1][:], AF.Sigmoid, [b_bank[B0 + 1]], b_sgb)
                    self.tt(t1, banks[B0 + 2][:], sga, ALU.mult, [b_bank[B0 + 2]] + b_sga, b_t1)
                    self.tt(t2, banks[B0 + 3][:], sgb, ALU.mult, [b_bank[B0 + 3]] + b_sgb, b_t2)
                    self.tt(mixedT[:, c, :], t1, t2, ALU.add, b_t1 + b_t2, b_mx, eng="gpsimd")
            fw.dma("gpsimd", lnp[:, 0:1024], lnp_d[0, :].partition_broadcast(128), writes=b_lnp)
            fw.dma("gpsimd", lnp[:, 1024:2048], lnp_d[1, :].partition_broadcast(128), writes=b_lnp)
            (wo0,), bwo0 = slab_load([(0, 8, 512, wb["w_out"][:, 0:512])])
            (wo1,), bwo1 = slab_load([(0, 8, 512, wb["w_out"][:, 512:1024])])
            def ld_xf(j):
                t0 = g * GT + j * 128
                fw.dma("gpsimd", xf, x[sq, t0:t0 + 128, :], writes=b_xf)

            def mm_out(j):
                for hh, (wo, bwo) in enumerate(((wo0, bwo0), (wo1, bwo1))):
                    bk = (j % 2) * 2 + hh
                    for k in range(8):
                        self.mm(banks[bk][:], mixedT[:, k, j * 128:(j + 1) * 128], wo[:, k, :], k == 0, k == 7, b_mx + [bwo], [b_bank[bk]])

            def post_out(j):
                for hh in range(2):
                    bk = (j % 2) * 2 + hh
                    self.stt(h1[:, j, hh * 512:(hh + 1) * 512], xf[:, hh * 512:(hh + 1) * 512], ALPHA, banks[bk][:], ALU.mult, ALU.add, b_xf + [b_bank[bk]], b_h1j[j])
                if j < 3:
                    ld_xf(j + 1)
                layer_norm_tile(h1[:, j, :], b_h1j[j], 0, h1[:, j, :], b_h1j[j], lnp[:, 0:1024], lnp[:, 1024:2048], b_lnp)
                self.vcopy("scalar", h1b, h1[:, j, :], b_h1j[j], b_h1b)
                for half in range(2):
                    bk = 6 + half
                    pv = banks[bk][:].bitcast(BF16)
                    for c4 in range(4):
                        c = half * 4 + c4
                        self.tr(pv[:, c4 * 128:(c4 + 1) * 128], h1b[:, c * 128:(c + 1) * 128], b_h1b, [b_bank[bk]], inc=(c4 == 3))
                    self.vcopy(evac_eng(), h1T[:, half * 4:(half + 1) * 4, j * 128:(j + 1) * 128], pv[:, 0:512].rearrange("p (c n) -> p c n", c=4), [b_bank[bk]], b_h1T)
            ld_xf(0)
            mm_out(0)
            mm_out(1)
            post_out(0)
            mm_out(2)
            post_out(1)
            mm_out(3)
            post_out(2)
            post_out(3)
            for d_ in range(3):
                issue_x(xpos[("B", sq, g, 3)] + 1 + d_)
            if g == 1:
                dbg_store("h1", h1[:, 0, :], b_h1)
            sl, b_sl = tv(68, 70, BF16)
            sl = [sl[:, 0:512], sl[:, 512:1024]]
            b_sl = [b_sl[0:1], b_sl[1:2]]
            for s in range(6):
                nc_ = 512 if s < 5 else 256
                (wg,), bwg = slab_load([(0, 8, nc_, wb["f_in"][:, s * 512:s * 512 + nc_])])
                (wu,), bwu = slab_load([(0, 8, nc_, wb["f_in"][:, DFF + s * 512:DFF + s * 512 + nc_])])
                for f4 in range(nc_ // 128):
                    f = s * 4 + f4
                    bg, bu = (0, 1) if f % 2 == 0 else (2, 3)
                    for k in range(8):
                        self.mm(banks[bg][:], wg[:, k, f4 * 128:(f4 + 1) * 128], h1T[:, k, :], k == 0, k == 7, [bwg] + b_h1T, [b_bank[bg]])
                    for k in range(8):
                        self.mm(banks[bu][:], wu[:, k, f4 * 128:(f4 + 1) * 128], h1T[:, k, :], k == 0, k == 7, [bwu] + b_h1T, [b_bank[bu]])
                    self.act(sl[f % 2], banks[bg][:], AF.Silu, [b_bank[bg]], b_sl[f % 2])
                    self.tt(actT[:, f, :], banks[bu][:], sl[f % 2], ALU.mult, [b_bank[bu]] + b_sl[f % 2], b_act[f:f + 1])
            for s in range(6):
                nf = 4 if s < 5 else 2
                (wd,), bwd = slab_load([(0, nf, 1024, wb["f_down"][s * 512:s * 512 + nf * 128, :])])
                for f4 in range(nf):
                    f = s * 4 + f4
                    for j in range(4):
                        for hh in range(2):
                            bk = j * 2 + hh
                            self.mm(banks[bk][:], actT[:, f, j * 128:(j + 1) * 128], wd[:, f4, hh * 512:(hh + 1) * 512], f == 0, f == NF - 1,
                                    b_act[f:f + 1] + [bwd], [b_bank[bk]], inc=(f == NF - 1 or (f4 == nf - 1 and j == 3 and hh == 1)))
            fw.dma("gpsimd", lnp[:, 0:1024], lnp_d[2, :].partition_broadcast(128), writes=b_lnp)
            fw.dma("gpsimd", lnp[:, 1024:2048], lnp_d[3, :].partition_broadcast(128), writes=b_lnp)
            ot, b_ot = tv(0, 8, F32)
            for j in (3, 2, 1, 0):
                z = ot[:, (j % 2) * 1024:(j % 2 + 1) * 1024]
                b_z = b_ot[(j % 2) * 4:(j % 2) * 4 + 4]
                for hh in range(2):
                    bk = j * 2 + hh
                    self.stt(z[:, hh * 512:(hh + 1) * 512], h1[:, j, hh * 512:(hh + 1) * 512], ALPHA, banks[bk][:], ALU.mult, ALU.add, b_h1j[j] + [b_bank[bk]], b_z)
                layer_norm_tile(z, b_z, 1, z, b_z, lnp[:, 0:1024], lnp[:, 1024:2048], b_lnp)
                t0 = g * GT + j * 128
                bo = Buf(f"out_{sq}_{g}_{j}")
                fw.dma("gpsimd", out[sq, t0:t0 + 128, :], z, reads=b_z, writes=[bo], group=b_z[0])
                b_outs.append(bo)

        try:
            self.chk(0)
            for sq in range(n_seq):
                for g in range(NG):
                    pass_a_group(sq, g)
                    if sq == 0 and g == 0:
                        prepass(["w_a", "w_b", "w_out", "f_in", "f_down"])
                    self.chk(4)
                dbg_store("oaT", o_aT[:].rearrange("p q n -> p (q n)"), b_oa)
                self.chk(5)
                for g in range(NG):
                    pass_b_group(sq, g)
                    self.chk(10)
        except StopBuild:
            pass
        if "cst" in dbg_out:
            dbg_store("cst", cstf[:, 0:128], [b_const])
        fw.wait_all("gpsimd", b_outs + list(b_w.values()))
        fw.emit()
        return nc


def host_inputs(inputs):
    w_in = np.ascontiguousarray(inputs["w_in"][0])
    w_uq = np.ascontiguousarray(inputs["mla_w_uq"][0])
    w_ukv = np.ascontiguousarray(inputs["mla_w_ukv"][0])
    w_krsw = np.zeros((1024, 96), np.float32)
    kr = w_in[:, C_KR:C_KR + 32]
    w_krsw[:, 64:80] = kr[:, 16:32]
    w_krsw[:, 80:96] = kr[:, 0:16]
    w_uqsw = np.zeros((256, 768), np.float32)
    for h in range(8):
        blk = w_uq[:, h * 96 + 64:h * 96 + 96]
        w_uqsw[:, h * 96 + 64:h * 96 + 80] = blk[:, 16:32]
        w_uqsw[:, h * 96 + 80:h * 96 + 96] = blk[:, 0:16]
    w_ki4 = np.ascontiguousarray(np.tile(w_in[:, C_KI:C_KI + 32], (1, 4)))
    kv = w_ukv.reshape(256, 8, 128)
    w_ukvk = np.ascontiguousarray(kv[:, :, 0:64].reshape(256, 512))
    w_ukvv = np.ascontiguousarray(kv[:, :, 64:128].reshape(256, 512))
    gq = np.ascontiguousarray(inputs["mla_q_norm"][0].reshape(2, 128).T)
    gkv = np.ascontiguousarray(inputs["mla_kv_norm"][0].reshape(2, 128).T)
    lnp = np.ascontiguousarray(np.stack([inputs["ln1_g"][0], inputs["ln1_b"][0], inputs["ln2_g"][0], inputs["ln2_b"][0]], 0))
    lnT = np.zeros((128, 16), np.float32)
    cst = np.zeros((128, 128 * 3 + NIT + 3), np.float32)
    cst[:, 0:128] = np.eye(128, dtype=np.float32)
    ii = np.arange(128)
    cst[:, 128:256] = (ii[None, :] >= ii[:, None]).astype(np.float32)
    cst[:, 256:384] = np.where(ii[None, :] <= ii[:, None], 0.0, -1e30)
    cst[:, 384:384 + NIT + 1] = (2.0 ** -(np.arange(NIT + 1) + 1.0))[None, :]
    inv_freq = (10000.0 ** (-np.arange(16, dtype=np.float32) / 16)).astype(np.float32)
    cst[64:80, 384 + NIT + 1] = inv_freq
    cst[80:96, 384 + NIT + 1] = inv_freq
    cst[:, 384 + NIT + 2] = 1.0
    cst[64:80, 384 + NIT + 2] = -1.0
    return {
        "w_in": w_in, "w_krsw": w_krsw, "w_ki4": w_ki4, "w_uq": w_uq, "w_uqsw": w_uqsw, "w_ukvk": w_ukvk, "w_ukvv": w_ukvv,
        "w_a": np.ascontiguousarray(inputs["w_branch_a"][0]), "w_b": np.ascontiguousarray(inputs["w_branch_b"][0]),
        "w_out": np.ascontiguousarray(inputs["w_out"][0]), "f_in": np.ascontiguousarray(inputs["ffn_w_in"][0]),
        "f_down": np.ascontiguousarray(inputs["ffn_w_down"][0]), "gq": gq, "gkv": gkv, "lnp": lnp, "lnT": lnT, "cst": cst,
    }


_NC_CACHE = {}


def kernel(**inputs):
    n_cores = 8
    n_seq = 4
    inputs = {k: np.asarray(v) for k, v in inputs.items()}
    shared = host_inputs(inputs)
    if n_seq not in _NC_CACHE:
        _NC_CACHE[n_seq] = K(n_seq).build()
    nc = _NC_CACHE[n_seq]
    x = inputs["x"]
    pos = inputs["positions"].astype(np.int32)
    in_maps = []
    for c in range(n_cores):
        m = dict(shared)
        m["x"] = np.ascontiguousarray(x[c * n_seq:(c + 1) * n_seq])
        m["pos"] = np.ascontiguousarray(pos[c * n_seq:(c + 1) * n_seq])
        in_maps.append(m)
    res = run_bass_kernel_spmd(nc, in_maps, core_ids=list(range(n_cores)))
    return np.concatenate([r["out"] for r in res.results], axis=0).astype(np.float32)
```

```python
import math
from contextlib import ExitStack
import numpy as np
import concourse.bass as bass
import concourse.mybir as mybir
from concourse.bass_utils import run_bass_kernel_spmd

F32 = mybir.dt.float32
BF16 = mybir.dt.bfloat16
I32 = mybir.dt.int32
ALU = mybir.AluOpType
AF = mybir.ActivationFunctionType
AX = mybir.AxisListType

D = 1024
S = 2048
NG = 4
GT = 512
DFF = 2816
NF = DFF // 128
DIN = 4424
ALPHA = 2.0 ** 0.25
LN_EPS = 1e-5
RMS_EPS = 1e-6
TOPK = 256
NIT = 17
SC_MLA = 1.0 / math.sqrt(96.0)
SC_DSA = 0.125
SLOPES = [2.0 ** (-(i + 1)) for i in range(8)]
C_CQ, C_CKV, C_KR, C_QB, C_KB, C_VB, C_QI, C_KI, C_WI, C_GA, C_GB = 0, 256, 512, 544, 1056, 1568, 2080, 2336, 2368, 2376, 3400
SEM_ROLL = 2000
SLABW = 4608


class Buf:
    __slots__ = ("name", "w", "r")

    def __init__(self, name):
        self.name = name
        self.w = None
        self.r = {}


class Eng:
    def __init__(self, name, eng):
        self.name = name
        self.eng = eng
        self.sem = None
        self.gen = 0
        self.n = 0
        self.seen = {}
        self.pending = False
        self.prog = []


class FW:
    def __init__(self, nc, stack):
        self.nc = nc
        self.stack = stack
        self.sems = {}
        self.E = {}
        self.dma_sem = {}

    def new_sem(self, name):
        self.sems[name] = self.stack.enter_context(self.nc.semaphore(name))
        return name

    def add_engine(self, name, eng):
        e = Eng(name, eng)
        e.sem = self.new_sem(f"p_{name}_0")
        self.E[name] = e

    def _collect(self, e, reads, writes):
        deps = {}
        for b in reads:
            if b.w is not None and deps.get(b.w[0], 0) < b.w[1]:
                deps[b.w[0]] = b.w[1]
        for b in writes:
            if b.w is not None and deps.get(b.w[0], 0) < b.w[1]:
                deps[b.w[0]] = b.w[1]
            for k, v in b.r.items():
                if deps.get(k, 0) < v:
                    deps[k] = v
        out = [(k, v) for k, v in deps.items() if e.seen.get(k, 0) < v and not (k == e.sem and v > e.n)
               and not (e.name == "tensor" and k.startswith("p_tensor"))]
        for k, v in out:
            e.seen[k] = v
        return out

    def _mark(self, d, reads, writes):
        for b in writes:
            b.w = d
            b.r = {}
        for b in reads:
            if b.r.get(d[0], 0) < d[1]:
                b.r[d[0]] = d[1]

    def op(self, ename, fn, reads=(), writes=(), inc=True):
        e = self.E[ename]
        if e.n >= SEM_ROLL and not e.pending:
            e.gen += 1
            e.sem = self.new_sem(f"p_{e.name}_{e.gen}")
            e.n = 0
        deps = self._collect(e, reads, writes)
        if inc:
            e.n += 1
            e.pending = False
            d = (e.sem, e.n)
        else:
            e.pending = True
            d = (e.sem, e.n + 1)
        incsem = e.sem if inc else None
        sems = self.sems

        def thunk(eng):
            for (k, v) in deps[1:]:
                eng.wait_ge(sems[k], v)
            ins = fn(eng)
            if deps:
                ins._wait_ge(sems[deps[0][0]], deps[0][1])
            if incsem is not None:
                ins.then_inc(sems[incsem], 1)
        e.prog.append(thunk)
        self._mark(d, reads, writes)

    def dma(self, ename, out, in_, reads=(), writes=(), group=None, **kw):
        e = self.E[ename]
        deps = self._collect(e, reads, writes)
        g = group if group is not None else (writes[0] if writes else reads[0])
        if g.name not in self.dma_sem:
            self.dma_sem[g.name] = [self.new_sem(f"d_{g.name}"), 0]
        rec = self.dma_sem[g.name]
        if rec[1] >= SEM_ROLL:
            self.dma_roll = getattr(self, "dma_roll", 0) + 1
            rec[0] = self.new_sem(f"d_{g.name}_{self.dma_roll}")
            rec[1] = 0
        rec[1] += 16
        d = (rec[0], rec[1])
        sems = self.sems
        sk = rec[0]

        def thunk(eng):
            for (k, v) in deps:
                eng.wait_ge(sems[k], v)
            eng.dma_start(out=out, in_=in_, **kw).then_inc(sems[sk], 16)
        e.prog.append(thunk)
        self._mark(d, reads, writes)

    def wait_all(self, ename, bufs):
        e = self.E[ename]
        deps = self._collect(e, bufs, ())
        sems = self.sems

        def thunk(eng):
            for (k, v) in deps:
                eng.wait_ge(sems[k], v)
        e.prog.append(thunk)

    def emit(self):
        with self.nc.Block() as block:
            for name, e in self.E.items():
                def body(eng, e=e):
                    for t in e.prog:
                        t(eng)
                getattr(block, name)(body)


class StopBuild(Exception):
    pass


class K:
    def chk(self, n):
        if self.stage == n:
            raise StopBuild()

    def __init__(self, n_seq, dbg=None, stage=99):
        self.n_seq = n_seq
        self.stage = stage
        self.dbg = dbg or {}
        self.nc = bass.Bass("TRN2", target_bir_lowering=False)
        self.act_rr = 0

    def mm(self, out, lhsT, rhs, start, stop, R, W, inc=None):
        if inc is None:
            inc = stop
        self.fw.op("tensor", lambda t: t.matmul(out, lhsT, rhs, start=start, stop=stop), reads=R, writes=W, inc=inc)

    def tr(self, out, in_, R, W, inc=True):
        idn = self.ident
        self.fw.op("tensor", lambda t: t.transpose(out, in_, idn), reads=R + [self.b_const], writes=W, inc=inc)

    def act(self, out, in_, func, R, W, bias=0.0, scale=1.0, accum=None):
        if accum is None:
            self.fw.op("scalar", lambda s: s.activation(out, in_, func, bias=bias, scale=scale), reads=R, writes=W)
        else:
            self.fw.op("scalar", lambda s: s.activation(out, in_, func, bias=bias, scale=scale, accum_out=accum), reads=R, writes=W)

    def vcopy(self, eng, out, in_, R, W):
        if eng == "scalar":
            self.fw.op("scalar", lambda s: s.copy(out, in_), reads=R, writes=W)
        else:
            self.fw.op(eng, lambda v: v.tensor_copy(out, in_), reads=R, writes=W)

    def tt(self, out, in0, in1, op, R, W, eng="vector"):
        self.fw.op(eng, lambda v: v.tensor_tensor(out, in0, in1, op), reads=R, writes=W)

    def ts(self, out, in0, s1, s2, op0, op1, R, W, accum=None, eng="vector"):
        if op1 is None:
            self.fw.op(eng, lambda v: v.tensor_scalar(out, in0, s1, None, op0), reads=R, writes=W)
        elif accum is None:
            self.fw.op(eng, lambda v: v.tensor_scalar(out, in0, s1, s2, op0, op1), reads=R, writes=W)
        else:
            self.fw.op(eng, lambda v: v.tensor_scalar(out, in0, s1, s2, op0, op1, accum_out=accum), reads=R, writes=W)

    def stt(self, out, in0, scalar, in1, op0, op1, R, W, accum=None):
        if accum is None:
            self.fw.op("vector", lambda v: v.scalar_tensor_tensor(out, in0, scalar, in1, op0, op1), reads=R, writes=W)
        else:
            self.fw.op("vector", lambda v: v.scalar_tensor_tensor(out, in0, scalar, in1, op0, op1, accum_out=accum), reads=R, writes=W)

    def recip(self, out, in_, R, W):
        self.fw.op("vector", lambda v: v.reciprocal(out, in_), reads=R, writes=W)

    def dram(self, name, shape, dt, kind):
        return self.nc.dram_tensor(name, shape, dt, kind=kind).ap()

    def build(self):
        nc = self.nc
        n_seq = self.n_seq
        st = self.stack = ExitStack()
        fw = self.fw = FW(nc, st)
        for n, e in [("sync", nc.sync), ("gpsimd", nc.gpsimd), ("scalar", nc.scalar), ("vector", nc.vector), ("tensor", nc.tensor)]:
            fw.add_engine(n, e)

        x = self.dram("x", [n_seq, S, D], F32, "ExternalInput")
        pos = self.dram("pos", [n_seq, S], I32, "ExternalInput")
        wspec = [("w_in", 1024, DIN), ("w_krsw", 1024, 96), ("w_ki4", 1024, 128), ("w_uq", 256, 768), ("w_uqsw", 256, 768),
                 ("w_ukvk", 256, 512), ("w_ukvv", 256, 512), ("w_a", 512, 1024), ("w_b", 512, 1024), ("w_out", 1024, 1024),
                 ("f_in", 1024, 2 * DFF), ("f_down", DFF, 1024)]
        wf, wb = {}, {}
        for nm, r, c in wspec:
            wf[nm] = self.dram(nm, [r, c], F32, "ExternalInput")
            wb[nm] = self.dram(nm + "_bf", [r, c], BF16, "Internal")
        gq_d = self.dram("gq", [128, 2], F32, "ExternalInput")
        gkv_d = self.dram("gkv", [128, 2], F32, "ExternalInput")
        lnp_d = self.dram("lnp", [4, D], F32, "ExternalInput")
        lnT_d = self.dram("lnT", [128, 16], F32, "ExternalInput")
        cst_d = self.dram("cst", [128, 128 * 3 + NIT + 3], F32, "ExternalInput")
        out = self.dram("out", [n_seq, S, D], F32, "ExternalOutput")
        dbg_out = {}
        for nm, shp in self.dbg.items():
            dbg_out[nm] = self.dram("dbg_" + nm, list(shp), F32, "ExternalOutput")
        self.dbg_out = dbg_out

        def sb(name, shape, dt):
            return st.enter_context(nc.sbuf_tensor("s_" + name, shape, dt))

        cstf = sb("cstf", [128, 128 * 3 + NIT + 3], F32)
        identb = sb("identb", [128, 128], BF16)
        tri01 = sb("tri01", [128, 128], BF16)
        onesb = sb("onesb", [128, 128], BF16)
        gq = sb("gq", [128, 2], F32)
        gkv = sb("gkv", [128, 2], F32)
        lnT = sb("lnT", [128, 16], F32)
        self.ident = identb[:]
        negtri = cstf[:, 256:384]
        pow2 = cstf[:, 384:384 + NIT + 1]
        freq = cstf[:, 384 + NIT + 1:384 + NIT + 2]
        sgn = cstf[:, 384 + NIT + 2:384 + NIT + 3]
        b_const = self.b_const = Buf("const")
        fw.dma("gpsimd", cstf[:], cst_d[:], writes=[b_const])
        fw.dma("gpsimd", gq[:], gq_d[:], writes=[b_const])
        fw.dma("gpsimd", gkv[:], gkv_d[:], writes=[b_const])
        fw.dma("gpsimd", lnT[:], lnT_d[:], writes=[b_const])
        self.vcopy("vector", identb[:], cstf[:, 0:128], [b_const], [b_const])
        self.vcopy("vector", tri01[:], cstf[:, 128:256], [b_const], [b_const])
        fw.op("vector", lambda v: v.memset(onesb[:], 1.0), writes=[b_const])

        b_w = {}
        dims = {nm: (r, c) for nm, r, c in wspec}
        for nm in dims:
            b_w[nm] = Buf("wscr_" + nm)

        def prepass(names):
            for nm in names:
                r, c = dims[nm]
                rows = (r * c) // 1024
                src = wf[nm].rearrange("r c -> (r c)").rearrange("(a b) -> a b", b=1024)
                dst = wb[nm].rearrange("r c -> (r c)").rearrange("(a b) -> a b", b=1024)
                a0 = 0
                while a0 < rows:
                    a1 = min(rows, a0 + 2048)
                    fw.dma("gpsimd", dst[a0:a1, :], src[a0:a1, :], writes=[Buf("wscr_tmp")], group=b_w[nm])
                    a0 = a1
                b_w[nm].w = tuple(fw.dma_sem[b_w[nm].name])
        prepass(["w_krsw", "w_uq", "w_uqsw", "w_ukvk", "w_ukvv", "w_ki4", "w_in"])
        self.b_w = b_w

        P_ELEMS = 28 * 1024
        Pa = sb("arenaP", [128, P_ELEMS], BF16)
        o_aT = sb("o_aT", [128, 4, S], BF16)
        T_ELEMS = 38 * 1024
        Ta = sb("arenaT", [128, T_ELEMS], BF16)
        NSLAB = 4
        slabs = [sb(f"slab{i}", [128, SLABW], BF16) for i in range(NSLAB)]
        b_slab = [Buf(f"slab{i}") for i in range(NSLAB)]
        self.slab_rr = 0
        small = sb("small", [128, 512], F32)
        b_small = Buf("small")

        banks = [st.enter_context(nc.psum_tensor(f"bank{i}", [128, 512], F32)) for i in range(8)]
        b_bank = [Buf(f"bank{i}") for i in range(8)]

        V_OFF = 16 * 1024
        Vt = Pa[:, V_OFF:V_OFF + 16 * 768].rearrange("p (c q e) -> p c q e", c=16, q=4)
        b_V = [Buf(f"V{g}") for g in range(NG)]
        b_V1 = Buf("Vones")
        fw.op("vector", lambda v: v.memset(Vt[:, :, :, 64:128], 1.0), writes=[b_V1])
        KA = [Pa[:, h * 2048:(h + 1) * 2048] for h in range(8)]
        b_KA = [[Buf(f"KA{h}_{g}") for g in range(NG)] for h in range(8)]
        KB = [Pa[:, p * 2048:(p + 1) * 2048] for p in range(4)]
        kiT = Pa[:, 4 * 2048:5 * 2048]
        b_KB = [b_KA[p] for p in range(4)]
        b_ki = b_KA[4]
        b_oa = [Buf(f"oa{g}") for g in range(NG)]

        RB = [Buf(f"T{i}") for i in range(76)]

        def tv(kb0, kb1, dt, shape=None):
            ap = Ta[:, kb0 * 512:kb1 * 512]
            if dt != BF16:
                ap = ap.bitcast(dt)
            return ap, RB[kb0:kb1]

        def slab_load(parts):
            i = self.slab_rr % NSLAB
            self.slab_rr += 1
            views = []
            for (off, kc, ncol, src) in parts:
                v = slabs[i][:, off:off + kc * ncol].rearrange("p (k n) -> p k n", k=kc)
                wname = src.tensor.name[:-3]
                fw.dma("sync", v, src.rearrange("(k p) n -> p k n", p=128), reads=[b_w[wname]], writes=[b_slab[i]])
                views.append(v)
            return views, b_slab[i]

        evac_rr = [0]

        def evac_eng():
            evac_rr[0] += 1
            return "scalar" if evac_rr[0] % 2 else "vector"

        b_outs = []

        def dbg_store(name, ap, bufs):
            if name in dbg_out:
                b = Buf("dbg_" + name)
                if ap.dtype != F32:
                    fw.dma("gpsimd", dbg_out[name][:], ap, reads=bufs, writes=[b], max_dma_last_dim=1024)
                else:
                    fw.dma("gpsimd", dbg_out[name][:], ap, reads=bufs, writes=[b])
                b_outs.append(b)

        xissued = set()
        xseq = []
        for sq_ in range(n_seq):
            for ps_ in ("A", "B"):
                for g_ in range(NG):
                    for j_ in range(4):
                        xseq.append((ps_, sq_, g_, j_))
        xpos = {k: i for i, k in enumerate(xseq)}

        def issue_x(idx):
            if idx >= len(xseq) or idx in xissued:
                return
            xissued.add(idx)
            ps_, sq_, g_, j_ = xseq[idx]
            xb, b_xb = tv(62 + 2 * (idx % 3), 64 + 2 * (idx % 3), BF16)
            t0 = g_ * GT + j_ * 128
            fw.dma("gpsimd", xb, x[sq_, t0:t0 + 128, :], writes=b_xb)

        def load_x_group(sq, g, ps):
            xT, b_xT = tv(32, 40, BF16)
            xT = xT.rearrange("p (k n) -> p k n", k=8)
            base = xpos[(ps, sq, g, 0)]
            for j in range(4):
                idx = base + j
                issue_x(idx)
                if j < 3:
                    issue_x(idx + 1)
                xb, b_xb = tv(62 + 2 * (idx % 3), 64 + 2 * (idx % 3), BF16)
                for half in range(2):
                    bk = 6 + half
                    pv = banks[bk][:].bitcast(BF16)
                    for c4 in range(4):
                        c = half * 4 + c4
                        self.tr(pv[:, c4 * 128:(c4 + 1) * 128], xb[:, c * 128:(c + 1) * 128], b_xb, [b_bank[bk]], inc=(c4 == 3))
                    src = pv[:, 0:512].rearrange("p (c n) -> p c n", c=4)
                    self.vcopy(evac_eng(), xT[:, half * 4:(half + 1) * 4, j * 128:(j + 1) * 128], src, [b_bank[bk]], b_xT)
            if ps == "A":
                for d_ in range(3):
                    issue_x(base + 4 + d_)
            return xT, b_xT

        def rope_tables(sq, g):
            posi, b_posi = tv(40, 42, I32)
            ang, b_ang = tv(42, 44, F32)
            tmp, b_tmp = tv(44, 46, F32)
            kk, b_kk = tv(46, 48, I32)
            cos, b_cos = tv(48, 50, F32)
            sin, b_sin = tv(50, 52, F32)
            r = slice(64, 96)
            fw.dma("gpsimd", posi[r, :], pos[sq, g * GT:(g + 1) * GT].partition_broadcast(32), writes=b_posi)
            self.vcopy("vector", tmp[r, :], posi[r, :], b_posi, b_tmp)
            self.ts(ang[r, :], tmp[r, :], freq[r, :], None, ALU.mult, None, b_tmp + [b_const], b_ang)
            for which, dst, b_dst, shift in (("sin", sin, b_sin, 0.0), ("cos", cos, b_cos, math.pi / 2)):
                self.ts(tmp[r, :], ang[r, :], shift, 1.0 / (2 * math.pi), ALU.add, ALU.mult, b_ang, b_tmp)
                self.vcopy("vector", kk[r, :], tmp[r, :], b_tmp, b_kk)
                self.vcopy("vector", tmp[r, :], kk[r, :], b_kk, b_tmp)
                self.ts(tmp[r, :], tmp[r, :], -2 * math.pi, shift, ALU.mult, ALU.add, b_tmp, b_tmp)
                self.tt(dst[r, :], tmp[r, :], ang[r, :], ALU.add, b_tmp + b_ang, b_dst)
                self.ts(tmp[r, :], dst[r, :], math.pi, -2 * math.pi, ALU.is_gt, ALU.mult, b_dst, b_tmp)
                self.tt(dst[r, :], dst[r, :], tmp[r, :], ALU.add, b_dst + b_tmp, b_dst)
                self.ts(tmp[r, :], dst[r, :], -math.pi, 2 * math.pi, ALU.is_lt, ALU.mult, b_dst, b_tmp)
                self.tt(dst[r, :], dst[r, :], tmp[r, :], ALU.add, b_dst + b_tmp, b_dst)
                self.ts(dst[r, :], dst[r, :], math.pi, -math.pi, ALU.min, ALU.max, b_dst, b_dst)
                if which == "sin":
                    self.act(dst[r, :], dst[r, :], AF.Sin, b_dst + [b_const], b_dst, scale=sgn[r, :])
                else:
                    self.act(dst[r, :], dst[r, :], AF.Sin, b_dst, b_dst)
            return cos, b_cos, sin, b_sin

        def rms_norm_fm(ps_list, gvec, outT, b_outT, sq_t, b_sq, rs, b_rs):
            for rr, bk in enumerate(ps_list):
                self.act(sq_t[:, rr, :], banks[bk][:], AF.Square, [b_bank[bk]], b_sq)
            for rr in range(2):
                self.mm(banks[5][:], onesb[:], sq_t[:, rr, :], rr == 0, rr == 1, b_sq + [b_const], [b_bank[5]])
            self.ts(rs, banks[5][:], 1.0 / 256.0, RMS_EPS, ALU.mult, ALU.add, [b_bank[5]], b_rs)
            self.act(rs, rs, AF.Sqrt, b_rs, b_rs)
            self.recip(rs, rs, b_rs, b_rs)
            for rr, bk in enumerate(ps_list):
                self.stt(outT[:, rr, :], banks[bk][:], gvec[:, rr:rr + 1], rs, ALU.mult, ALU.mult, [b_bank[bk], b_const] + b_rs, b_outT)

        def attention(g, nheads, kfn, qfn, vsel, scale, oT, b_oT, bias_fn=None, mask=None):
            NPT, NS, LA = 8, 4, 3
            PT = [tv(68 + i, 69 + i, BF16) for i in range(NPT)]
            SBK = [2, 3, 6, 7]
            rd, b_rd = tv(56, 58, F32)
            nch = 4 * g + 4
            items = [(h, c) for h in range(nheads) for c in range(nch)]

            def emit_score(idx):
                h, c = items[idx]
                n0 = 0 if c < 4 * g else (c - 4 * g) * 128
                sbk = SBK[idx % NS]
                pt, b_pt = PT[idx % NPT]
                kap, kb = kfn(h, c)
                qap, qb = qfn(h, n0)
                self.mm(banks[sbk][:, n0:512], kap, qap, True, True, kb + qb, [b_bank[sbk]])
                if bias_fn is None:
                    self.act(pt[:, n0:512], banks[sbk][:, n0:512], AF.Exp, [b_bank[sbk]], b_pt, scale=scale)
                else:
                    for (b0, b1, bias_ap, bb) in bias_fn(h, c, n0):
                        self.act(pt[:, b0:b1], banks[sbk][:, b0:b1], AF.Exp, [b_bank[sbk]] + bb, b_pt, bias=bias_ap, scale=scale)
                if mask is not None:
                    map_, mb = mask(c, n0)
                    self.tt(pt[:, n0:512], pt[:, n0:512], map_, ALU.mult, b_pt + mb, b_pt)
                elif c >= 4 * g:
                    self.tt(pt[:, n0:n0 + 128], pt[:, n0:n0 + 128], tri01[:], ALU.mult, b_pt + [b_const], b_pt)

            def emit_pv(idx):
                h, c = items[idx]
                n0 = 0 if c < 4 * g else (c - 4 * g) * 128
                acc = 4 + (h % 2)
                pt, b_pt = PT[idx % NPT]
                vap, vb = vsel(h, c)
                self.mm(banks[acc][:, n0:512], vap, pt[:, n0:512], c == 0, c == nch - 1, vb + b_pt, [b_bank[acc]], inc=(c == nch - 1))
                if c == nch - 1:
                    pr = h // 2
                    if h % 2 == 0:
                        if mask is None:
                            self.act(rd[0:64, :], banks[acc][64:128, :], AF.Ln, [b_bank[acc]], b_rd)
                            self.act(rd[0:64, :], rd[0:64, :], AF.Exp, b_rd, b_rd, scale=-1.0)
                        else:
                            self.recip(rd[0:64, :], banks[acc][64:128, :], [b_bank[acc]], b_rd)
                        self.tt(oT[0:64, pr, :], banks[acc][0:64, :], rd[0:64, :], ALU.mult, [b_bank[acc]] + b_rd, b_oT)
                    else:
                        if mask is None:
                            self.act(rd[64:128, :], banks[acc][0:64, :], AF.Ln, [b_bank[acc]], b_rd)
                            self.act(rd[64:128, :], rd[64:128, :], AF.Exp, b_rd, b_rd, scale=-1.0)
                        else:
                            self.recip(rd[64:128, :], banks[acc][0:64, :], [b_bank[acc]], b_rd)
                        self.tt(oT[64:128, pr, :], banks[acc][64:128, :], rd[64:128, :], ALU.mult, [b_bank[acc]] + b_rd, b_oT)

            for idx in range(len(items) + LA):
                if idx < len(items):
                    emit_score(idx)
                if idx - LA >= 0:
                    emit_pv(idx - LA)

        def pass_a_group(sq, g):
            gs = slice(g * GT, (g + 1) * GT)
            xT, b_xT = load_x_group(sq, g, "A")
            dbg_store("xT", xT.rearrange("p k n -> p (k n)"), b_xT)
            self.chk(1)
            (w1,), bw1 = slab_load([(0, 8, 544, wb["w_in"][:, 0:544])])
            (wkrsw, wuq, wuqsw), bw2 = slab_load([(0, 8, 96, wb["w_krsw"][:, :]), (768, 2, 768, wb["w_uq"][:, :]), (2304, 2, 768, wb["w_uqsw"][:, :])])
            (wukvk, wukvv), bw3 = slab_load([(0, 2, 512, wb["w_ukvk"][:, :]), (1024, 2, 512, wb["w_ukvv"][:, :])])
            cos, b_cos, sin, b_sin = rope_tables(sq, g)
            cqT, b_cq = tv(0, 2, BF16)
            cqT = cqT.rearrange("p (r n) -> p r n", r=2)
            ckvT, b_ckv = tv(2, 4, BF16)
            ckvT = ckvT.rearrange("p (r n) -> p r n", r=2)
            sq_t, b_sq = tv(4, 6, BF16)
            sq_t = sq_t.rearrange("p (r n) -> p r n", r=2)
            rs, b_rs = tv(6, 8, F32)
            t1, b_t1 = tv(8, 10, F32)
            t2, b_t2 = tv(10, 12, F32)
            for which, col0, outT, b_o, gvec in (("q", C_CQ, cqT, b_cq, gq), ("kv", C_CKV, ckvT, b_ckv, gkv)):
                for rr in range(2):
                    for k in range(8):
                        self.mm(banks[rr][:], w1[:, k, col0 + rr * 128:col0 + (rr + 1) * 128], xT[:, k, :], k == 0, k == 7, [bw1] + b_xT, [b_bank[rr]])
                rms_norm_fm([0, 1], gvec, outT, b_o, sq_t, b_sq, rs, b_rs)
            for k in range(8):
                self.mm(banks[0][0:96, :], w1[:, k, 448:544], xT[:, k, :], k == 0, k == 7, [bw1] + b_xT, [b_bank[0]])
            for k in range(8):
                self.mm(banks[1][0:96, :], wkrsw[:, k, :], xT[:, k, :], k == 0, k == 7, [bw2] + b_xT, [b_bank[1]])
            r = slice(64, 96)
            self.tt(t1[r, :], banks[0][r, :], cos[r, :], ALU.mult, [b_bank[0]] + b_cos, b_t1)
            self.tt(t2[r, :], banks[1][r, :], sin[r, :], ALU.mult, [b_bank[1]] + b_sin, b_t2)
            for h in range(8):
                self.tt(KA[h][r, gs], t1[r, :], t2[r, :], ALU.add, b_t1 + b_t2, [b_KA[h][g]])
            for pr in range(4):
                bk = pr
                for rr in range(2):
                    self.mm(banks[bk][:], wukvk[:, rr, pr * 128:(pr + 1) * 128], ckvT[:, rr, :], rr == 0, rr == 1, [bw3] + b_ckv, [b_bank[bk]])
                self.vcopy("scalar", KA[2 * pr][0:64, gs], banks[bk][0:64, :], [b_bank[bk]], [b_KA[2 * pr][g]])
                self.vcopy("vector", KA[2 * pr + 1][0:64, gs], banks[bk][64:128, :], [b_bank[bk]], [b_KA[2 * pr + 1][g]])
            for j in range(4):
                bk = j
                for rr in range(2):
                    self.mm(banks[bk][:], ckvT[:, rr, j * 128:(j + 1) * 128], wukvv[:, rr, :], rr == 0, rr == 1, [bw3] + b_ckv, [b_bank[bk]])
                src = banks[bk][:].rearrange("p (q e d) -> p q e d", q=4, e=2)
                self.vcopy("scalar", Vt[:, g * 4 + j, :, 0:64], src[:, :, 0, :], [b_bank[bk]], [b_V[g]])
                self.vcopy("vector", Vt[:, g * 4 + j, :, 128:192], src[:, :, 1, :], [b_bank[bk]], [b_V[g]])
            dbg_store("cos", cos[64:96, :], b_cos)
            dbg_store("sin", sin[64:96, :], b_sin)
            self.chk(2)
            QA, b_QA = tv(12, 20, BF16)
            QA = QA.rearrange("p (h n) -> p h n", h=8)
            b_QAh = [b_QA[h:h + 1] for h in range(8)]
            for h in range(8):
                ba, bb_ = (0, 1) if h % 2 == 0 else (4, 5)
                for rr in range(2):
                    self.mm(banks[ba][0:96, :], wuq[:, rr, h * 96:(h + 1) * 96], cqT[:, rr, :], rr == 0, rr == 1, [bw2] + b_cq, [b_bank[ba]])
                for rr in range(2):
                    self.mm(banks[bb_][0:96, :], wuqsw[:, rr, h * 96:(h + 1) * 96], cqT[:, rr, :], rr == 0, rr == 1, [bw2] + b_cq, [b_bank[bb_]])
                self.vcopy("scalar", QA[0:64, h, :], banks[ba][0:64, :], [b_bank[ba]], b_QAh[h])
                self.tt(t1[r, :], banks[ba][r, :], cos[r, :], ALU.mult, [b_bank[ba]] + b_cos, b_t1)
                self.tt(t2[r, :], banks[bb_][r, :], sin[r, :], ALU.mult, [b_bank[bb_]] + b_sin, b_t2)
                self.tt(QA[r, h, :], t1[r, :], t2[r, :], ALU.add, b_t1 + b_t2, b_QAh[h])
            if g == 1:
                dbg_store("QA0", QA[0:96, 0, :], b_QA)
                dbg_store("KA0", KA[0][0:96, 0:1024], [b_KA[0][0], b_KA[0][1]])
                dbg_store("cqT", cqT.rearrange("p r n -> p (r n)"), b_cq)

            self.chk(3)

            def kfn(h, c):
                return KA[h][0:96, c * 128:(c + 1) * 128], [b_KA[h][c // 4]]

            def qfn(h, n0):
                return QA[0:96, h, n0:512], b_QAh[h]

            def vsel(h, c):
                pr = h // 2
                if h % 2 == 0:
                    return Vt[:, c, pr, 0:128], [b_V[c // 4], b_V1]
                return Vt[:, c, pr, 64:192], [b_V[c // 4], b_V1]
            attention(g, 8, kfn, qfn, vsel, SC_MLA, o_aT[:, :, gs], [b_oa[g]])

        def pass_b_group(sq, g):
            gs = slice(g * GT, (g + 1) * GT)
            xT, b_xT = load_x_group(sq, g, "B")
            (wqb,), bwq = slab_load([(0, 8, 512, wb["w_in"][:, C_QB:C_QB + 512])])
            (wkb,), bwk = slab_load([(0, 8, 512, wb["w_in"][:, C_KB:C_KB + 512])])
            (wvb,), bwv = slab_load([(0, 8, 512, wb["w_in"][:, C_VB:C_VB + 512])])
            (wqi, wki, wwi), bwi = slab_load([(0, 8, 256, wb["w_in"][:, C_QI:C_QI + 256]), (2048, 8, 128, wb["w_ki4"][:, :]),
                                              (3072, 8, 8, wb["w_in"][:, C_WI:C_WI + 8])])
            QB, b_QB = tv(0, 4, BF16)
            QB = QB.rearrange("p (q n) -> p q n", q=4)
            qiT, b_qi = tv(4, 6, BF16)
            qiT = qiT.rearrange("p (q n) -> p q n", q=2)
            for pr in range(4):
                bk = pr
                for k in range(8):
                    self.mm(banks[bk][:], wqb[:, k, pr * 128:(pr + 1) * 128], xT[:, k, :], k == 0, k == 7, [bwq] + b_xT, [b_bank[bk]])
                self.vcopy(evac_eng(), QB[:, pr, :], banks[bk][:], [b_bank[bk]], b_QB)
            for pr in range(4):
                bk = pr
                for k in range(8):
                    self.mm(banks[bk][:], wkb[:, k, pr * 128:(pr + 1) * 128], xT[:, k, :], k == 0, k == 7, [bwk] + b_xT, [b_bank[bk]])
                self.vcopy(evac_eng(), KB[pr][:, gs], banks[bk][:], [b_bank[bk]], [b_KB[pr][g]])
            for j in range(4):
                bk = j
                for k in range(8):
                    self.mm(banks[bk][:], xT[:, k, j * 128:(j + 1) * 128], wvb[:, k, :], k == 0, k == 7, [bwv] + b_xT, [b_bank[bk]])
                src = banks[bk][:].rearrange("p (q e d) -> p q e d", q=4, e=2)
                self.vcopy("scalar", Vt[:, g * 4 + j, :, 0:64], src[:, :, 0, :], [b_bank[bk]], [b_V[g]])
                self.vcopy("vector", Vt[:, g * 4 + j, :, 128:192], src[:, :, 1, :], [b_bank[bk]], [b_V[g]])
            for q2 in range(2):
                bk = q2
                for k in range(8):
                    self.mm(banks[bk][:], wqi[:, k, q2 * 128:(q2 + 1) * 128], xT[:, k, :], k == 0, k == 7, [bwi] + b_xT, [b_bank[bk]])
                self.vcopy(evac_eng(), qiT[:, q2, :], banks[bk][:], [b_bank[bk]], b_qi)
            for k in range(8):
                self.mm(banks[0][:], wki[:, k, :], xT[:, k, :], k == 0, k == 7, [bwi] + b_xT, [b_bank[0]])
            self.vcopy(evac_eng(), kiT[:, gs], banks[0][:], [b_bank[0]], [b_ki[g]])
            wI = small[:, 0:32].rearrange("p (j h) -> p j h", j=4)
            for j in range(4):
                for k in range(8):
                    self.mm(banks[1][:, j * 8:(j + 1) * 8], xT[:, k, j * 128:(j + 1) * 128], wwi[:, k, :], k == 0, k == 7, [bwi] + b_xT, [b_bank[1]])
            self.vcopy("vector", small[:, 0:32], banks[1][:, 0:32], [b_bank[1]], [b_small])
            self.chk(6)
            posT_i = small[:, 64:80].bitcast(I32)
            posT = small[:, 80:96]
            posq_i = small[:, 96:100].bitcast(I32)
            posq = small[:, 100:104]
            fw.dma("gpsimd", posT_i, pos[sq, :].rearrange("(c p) -> p c", p=128), writes=[b_small], allow_slow_non_contiguous=True)
            fw.dma("gpsimd", posq_i, pos[sq, g * GT:(g + 1) * GT].rearrange("(j p) -> j p", p=128)[:, 0].partition_broadcast(128), writes=[b_small], allow_slow_non_contiguous=True)
            self.vcopy("vector", posT, posT_i, [b_small], [b_small])
            self.vcopy("vector", posq, posq_i, [b_small], [b_small])
            biasall = small[:, 128:192].rearrange("p (c j) -> p c j", c=16)
            self.tt(biasall, posT.unsqueeze(2).to_broadcast([128, 16, 4]), posq.unsqueeze(1).to_broadcast([128, 16, 4]), ALU.subtract, [b_small], [b_small])
            biash = [small[:, 192 + 0:192 + 64], small[:, 256:320]]
            b_biash = [Buf("biash0"), Buf("biash1")]

            R, b_R = tv(6, 14, BF16)
            R = R.rearrange("p (h n) -> p h n", h=8)
            scsL = [tv(14, 22, F32), tv(68, 76, F32)]
            mskL = [tv(22, 26, BF16), tv(6, 10, BF16)]
            maskT, b_maskT = tv(40, 56, BF16)
            maskT = maskT.rearrange("p (c n) -> p c n", c=16)
            Dw, b_Dw = tv(26, 28, BF16)
            Dw = Dw.rearrange("p (h n) -> p h n", h=8)
            b_ch = [Buf(f"chain{q}_{sq}_{g}") for q in range(2)]
            chs = []
            for q in range(2):
                bis = small[:, 320 + 16 * q:336 + 16 * q]
                chs.append(dict(A=bis[:, 0:1], W=bis[:, 1:2], mid=bis[:, 2:3], cnt=bis[:, 3:4], gs=bis[:, 4:5], thr=bis[:, 5:6], mid2=bis[:, 6:7],
                                steps=small[:, 352 + q * (NIT + 1):352 + (q + 1) * (NIT + 1)], b=[b_ch[q]]))
            for jp in (0, 2):
                info = []
                for q in range(2):
                    j = jp + q
                    qb = 4 * g + j
                    svis = (qb + 1) * 128
                    scs, b_scs = scsL[q]
                    ch = chs[q]
                    for h in range(8):
                        self.ts(Dw[:, h, :], identb[:], wI[:, j, h:h + 1], None, ALU.mult, None, [b_const, b_small], b_Dw)
                    nsg = (svis + 511) // 512
                    for sg in range(nsg):
                        ncol = min(512, svis - sg * 512)
                        scb = 2 if sg % 2 == 0 else 5
                        for h in range(8):
                            bk = (0, 1, 3, 4)[h % 4]
                            rows = slice((h % 4) * 32, (h % 4) * 32 + 32)
                            tp = ((h % 4) * 32, 0)
                            lhs = qiT[rows, h // 4, j * 128:(j + 1) * 128]
                            rhs = kiT[rows, sg * 512:sg * 512 + ncol]
                            fw.op("tensor", (lambda o, l, r_, tp_: (lambda t: t.matmul(o, l, r_, start=True, stop=True, tile_position=tp_)))(banks[bk][:, 0:ncol], lhs, rhs, tp),
                                  reads=b_qi + [b_ki[sg]], writes=[b_bank[bk]])
                            if h % 2:
                                self.act(R[:, h, 0:ncol], banks[bk][:, 0:ncol], AF.Relu, [b_bank[bk]], b_R[h:h + 1])
                            else:
                                self.ts(R[:, h, 0:ncol], banks[bk][:, 0:ncol], 0.0, None, ALU.max, None, [b_bank[bk]], b_R[h:h + 1])
                        for h in range(8):
                            self.mm(banks[scb][:, 0:ncol], Dw[:, h, :], R[:, h, 0:ncol], h == 0, h == 7, b_Dw + b_R[h:h + 1], [b_bank[scb]])
                        self.vcopy("scalar", scs[:, sg * 512:sg * 512 + ncol], banks[scb][:, 0:ncol], [b_bank[scb]], b_scs)
                    dcol = slice(qb * 128, (qb + 1) * 128)
                    if qb >= 2:
                        fw.op("vector", lambda v, o=ch["A"], i=scs[:, 0:svis]: v.tensor_reduce(o, i, AX.X, ALU.max, apply_absolute_value=True), reads=b_scs, writes=ch["b"])
                    self.tt(scs[:, dcol], scs[:, dcol], negtri, ALU.add, b_scs + [b_const], b_scs)
                    info.append((j, qb, svis))
                if info[0][1] >= 2:
                    for q in range(2):
                        ch = chs[q]
                        sgn_ = 1.0 if q == 0 else -1.0
                        self.ts(ch["W"], ch["A"], 2.0 * sgn_, 1e-3 * sgn_, ALU.mult, ALU.add, ch["b"], ch["b"])
                        self.ts(ch["steps"], pow2, ch["W"], None, ALU.mult, None, ch["b"] + [b_const], ch["b"])
                        self.stt(ch["mid"], ch["A"], -1.0 * sgn_, ch["steps"][:, 0:1], ALU.mult, ALU.add, ch["b"], ch["b"])
                        self.ts(ch["mid"], ch["mid"], -5e-4 * sgn_, None, ALU.add, None, ch["b"], ch["b"])
                    for i in range(NIT):
                        ch = chs[0]
                        j, qb, svis = info[0]
                        scs, b_scs = scsL[0]
                        jk, b_jk = mskL[0]
                        self.ts(jk[:, 0:svis], scs[:, 0:svis], ch["mid"], None, ALU.is_ge, ALU.add, b_scs + ch["b"], b_jk + ch["b"], accum=ch["cnt"])
                        self.ts(ch["gs"], ch["cnt"], TOPK - 0.5, ch["steps"][:, i:i + 1], ALU.is_ge, ALU.mult, ch["b"], ch["b"])
                        if i < NIT - 1:
                            self.stt(ch["mid"], ch["mid"], ch["steps"][:, i + 1:i + 2], ch["gs"], ALU.subtract, ALU.add, ch["b"], ch["b"])
                        else:
                            self.stt(ch["thr"], ch["mid"], ch["steps"][:, i:i + 1], ch["gs"], ALU.subtract, ALU.add, ch["b"], ch["b"])
                        ch = chs[1]
                        j, qb, svis = info[1]
                        scs, b_scs = scsL[1]
                        jk, b_jk = mskL[1]
                        cur = ch["mid"] if i % 2 == 0 else ch["mid2"]
                        nxt = ch["mid2"] if i % 2 == 0 else ch["mid"]
                        self.act(jk[:, 0:svis], scs[:, 0:svis], AF.Sign, b_scs + ch["b"], b_jk + ch["b"], bias=cur, scale=1.0, accum=ch["cnt"])
                        self.act(ch["gs"], ch["cnt"], AF.Sign, ch["b"], ch["b"], bias=-(2.0 * TOPK - svis - 0.5), scale=1.0)
                        self.act(nxt, ch["gs"], AF.Identity, ch["b"], ch["b"], bias=cur, scale=ch["steps"][:, i + 1:i + 2])
                    ch = chs[1]
                    fin = ch["mid"] if NIT % 2 == 0 else ch["mid2"]
                    self.ts(ch["thr"], fin, -1.0, ch["steps"][:, NIT:NIT + 1], ALU.mult, ALU.add, ch["b"], ch["b"])
                for q in range(2):
                    ch = chs[q]
                    j, qb, svis = info[q]
                    scs, b_scs = scsL[q]
                    mask, b_mask = mskL[q]
                    if qb >= 2:
                        self.ts(mask[:, 0:svis], scs[:, 0:svis], ch["thr"], None, ALU.is_ge, None, b_scs + ch["b"], b_mask)
                    else:
                        self.ts(mask[:, 0:svis], scs[:, 0:svis], -1e29, None, ALU.is_ge, None, b_scs, b_mask)
                    c = 0
                    while c <= qb:
                        n = min(8, qb + 1 - c)
                        bk = 6 + ((c // 8) % 2)
                        pv = banks[bk][:].bitcast(BF16)
                        for i in range(n):
                            self.tr(pv[:, i * 128:(i + 1) * 128], mask[:, (c + i) * 128:(c + i + 1) * 128], b_mask, [b_bank[bk]], inc=(i == n - 1))
                        self.vcopy(evac_eng(), maskT[:, c:c + n, j * 128:(j + 1) * 128], pv[:, 0:n * 128].rearrange("p (c n) -> p c n", c=n), [b_bank[bk]], b_maskT)
                        c += n
            if g == 1:
                dbg_store("maskT", maskT[:, 0:8, :].rearrange("p c n -> p (c n)"), b_maskT)

            self.chk(7)
            o_bT, b_ob = tv(58, 62, BF16)
            o_bT = o_bT.rearrange("p (q n) -> p q n", q=4)
            cur = {"h": -1}

            def bias_fn(h, c, n0):
                k = h % 2
                if cur["h"] != h:
                    cur["h"] = h
                    self.ts(biash[k], small[:, 128:192], SLOPES[h], None, ALU.mult, None, [b_small], [b_biash[k]])
                span = 128 if h == 0 else (256 if h == 1 else 512)
                res = []
                b0 = n0
                while b0 < 512:
                    blk_start = (b0 // span) * span
                    b1 = min(512, blk_start + span)
                    jj = blk_start // 128
                    res.append((b0, b1, biash[k].rearrange("p (c j) -> p c j", c=16)[:, c, jj:jj + 1], [b_biash[k]]))
                    b0 = b1
                return res

            def kfn(h, c):
                return KB[h // 2][(h % 2) * 64:(h % 2) * 64 + 64, c * 128:(c + 1) * 128], [b_KB[h // 2][c // 4]]

            def qfn(h, n0):
                return QB[(h % 2) * 64:(h % 2) * 64 + 64, h // 2, n0:512], b_QB

            def vsel(h, c):
                pr = h // 2
                if h % 2 == 0:
                    return Vt[:, c, pr, 0:128], [b_V[c // 4], b_V1]
                return Vt[:, c, pr, 64:192], [b_V[c // 4], b_V1]

            def maskfn(c, n0):
                return maskT[:, c, n0:512], b_maskT
            attention(g, 8, kfn, qfn, vsel, SC_DSA, o_bT, b_ob, bias_fn=bias_fn, mask=maskfn)
            if g == 1:
                dbg_store("obT", o_bT.rearrange("p q n -> p (q n)"), b_ob)
            self.chk(8)
            phase2(sq, g, xT, b_xT, o_bT, b_ob)

        def layer_norm_tile(z, b_z, gsel, outap, b_outap, lng, lnb, b_ln):
            stats = small[:, 400:412].rearrange("p (c s) -> p c s", c=2)
            mv = small[:, 412:414]
            rstd = small[:, 414:415]
            for hh in range(2):
                fw.op("vector", lambda v, o=stats[:, hh, :], i=z[:, hh * 512:(hh + 1) * 512]: v.bn_stats(o, i), reads=b_z, writes=[b_small])
            fw.op("vector", lambda v: v.bn_aggr(mv, small[:, 400:412]), reads=[b_small], writes=[b_small])
            self.ts(rstd, mv[:, 1:2], LN_EPS, None, ALU.add, None, [b_small], [b_small])
            self.act(rstd, rstd, AF.Sqrt, [b_small], [b_small])
            self.recip(rstd, rstd, [b_small], [b_small])
            self.ts(z, z, mv[:, 0:1], rstd, ALU.subtract, ALU.mult, b_z + [b_small], b_z)
            self.tt(z, z, lng, ALU.mult, b_z + b_ln, b_z)
            self.tt(outap, z, lnb, ALU.add, b_z + b_ln, b_outap)

        def phase2(sq, g, xT, b_xT, o_bT, b_ob):
            gs = slice(g * GT, (g + 1) * GT)
            mixedT, b_mx = tv(0, 8, BF16)
            mixedT = mixedT.rearrange("p (k n) -> p k n", k=8)
            lnp, b_lnp = tv(8, 16, F32)
            h1, b_h1 = tv(16, 32, F32)
            h1 = h1.rearrange("p (j n) -> p j n", j=4)
            b_h1j = [b_h1[4 * j:4 * j + 4] for j in range(4)]
            h1T, b_h1T = tv(32, 40, BF16)
            h1T = h1T.rearrange("p (k n) -> p k n", k=8)
            actT, b_act = tv(40, 62, BF16)
            actT = actT.rearrange("p (f n) -> p f n", f=NF)
            xf, b_xf = tv(62, 66, F32)
            h1b, b_h1b = tv(66, 68, BF16)
            sga, b_sga = tv(68, 69, BF16)
            sgb, b_sgb = tv(69, 70, BF16)
            t1, b_t1 = tv(70, 72, F32)
            t2, b_t2 = tv(72, 74, F32)
            for half in range(2):
                (wa,), bwa = slab_load([(0, 4, 1024, wb["w_a"][:, :])])
                (wb_,), bwb = slab_load([(0, 4, 1024, wb["w_b"][:, :])])
                (wga,), bga = slab_load([(0, 8, 512, wb["w_in"][:, C_GA + half * 512:C_GA + (half + 1) * 512])])
                (wgb,), bgb = slab_load([(0, 8, 512, wb["w_in"][:, C_GB + half * 512:C_GB + (half + 1) * 512])])
                for c4 in range(4):
                    c = half * 4 + c4
                    B0 = (c % 2) * 4
                    for k in range(8):
                        self.mm(banks[B0][:], wga[:, k, c4 * 128:(c4 + 1) * 128], xT[:, k, :], k == 0, k == 7, [bga] + b_xT, [b_bank[B0]])
                    for k in range(8):
                        self.mm(banks[B0 + 1][:], wgb[:, k, c4 * 128:(c4 + 1) * 128], xT[:, k, :], k == 0, k == 7, [bgb] + b_xT, [b_bank[B0 + 1]])
                    for p in range(4):
                        self.mm(banks[B0 + 2][:], wa[:, p, c * 128:(c + 1) * 128], o_aT[:, p, gs], p == 0, p == 3, [bwa, b_oa[g]], [b_bank[B0 + 2]])
                    for p in range(4):
                        self.mm(banks[B0 + 3][:], wb_[:, p, c * 128:(c + 1) * 128], o_bT[:, p, :], p == 0, p == 3, [bwb] + b_ob, [b_bank[B0 + 3]])
                    self.act(sga, banks[B0][:], AF.Sigmoid, [b_bank[B0]], b_sga)
                    self.act(sgb, banks[B0 + 1][:], AF.Sigmoid, [b_bank[B0 + 1]], b_sgb)
                    self.tt(t1, banks[B0 + 2][:], sga, ALU.mult, [b_bank[B0 + 2]] + b_sga, b_t1)
                    self.tt(t2, banks[B0 + 3][:], sgb, ALU.mult, [b_bank[B0 + 3]] + b_sgb, b_t2)
                    self.tt(mixedT[:, c, :], t1, t2, ALU.add, b_t1 + b_t2, b_mx, eng="gpsimd")
            fw.dma("gpsimd", lnp[:, 0:1024], lnp_d[0, :].partition_broadcast(128), writes=b_lnp)
            fw.dma("gpsimd", lnp[:, 1024:2048], lnp_d[1, :].partition_broadcast(128), writes=b_lnp)
            (wo0,), bwo0 = slab_load([(0, 8, 512, wb["w_out"][:, 0:512])])
            (wo1,), bwo1 = slab_load([(0, 8, 512, wb["w_out"][:, 512:1024])])
            def ld_xf(j):
                t0 = g * GT + j * 128
                fw.dma("gpsimd", xf, x[sq, t0:t0 + 128, :], writes=b_xf)

            def mm_out(j):
                for hh, (wo, bwo) in enumerate(((wo0, bwo0), (wo1, bwo1))):
                    bk = (j % 2) * 2 + hh
                    for k in range(8):
                        self.mm(banks[bk][:], mixedT[:, k, j * 128:(j + 1) * 128], wo[:, k, :], k == 0, k == 7, b_mx + [bwo], [b_bank[bk]])

            def post_out(j):
                for hh in range(2):
                    bk = (j % 2) * 2 + hh
                    self.stt(h1[:, j, hh * 512:(hh + 1) * 512], xf[:, hh * 512:(hh + 1) * 512], ALPHA, banks[bk][:], ALU.mult, ALU.add, b_xf + [b_bank[bk]], b_h1j[j])
                if j < 3:
                    ld_xf(j + 1)
                layer_norm_tile(h1[:, j, :], b_h1j[j], 0, h1[:, j, :], b_h1j[j], lnp[:, 0:1024], lnp[:, 1024:2048], b_lnp)
                self.vcopy("scalar", h1b, h1[:, j, :], b_h1j[j], b_h1b)
                for half in range(2):
                    bk = 6 + half
                    pv = banks[bk][:].bitcast(BF16)
                    for c4 in range(4):
                        c = half * 4 + c4
                        self.tr(pv[:, c4 * 128:(c4 + 1) * 128], h1b[:, c * 128:(c + 1) * 128], b_h1b, [b_bank[bk]], inc=(c4 == 3))
                    self.vcopy(evac_eng(), h1T[:, half * 4:(half + 1) * 4, j * 128:(j + 1) * 128], pv[:, 0:512].rearrange("p (c n) -> p c n", c=4), [b_bank[bk]], b_h1T)
            ld_xf(0)
            mm_out(0)
            mm_out(1)
            post_out(0)
            mm_out(2)
            post_out(1)
            mm_out(3)
            post_out(2)
            post_out(3)
            for d_ in range(3):
                issue_x(xpos[("B", sq, g, 3)] + 1 + d_)
            if g == 1:
                dbg_store("h1", h1[:, 0, :], b_h1)
            sl, b_sl = tv(68, 70, BF16)
            sl = [sl[:, 0:512], sl[:, 512:1024]]
            b_sl = [b_sl[0:1], b_sl[1:2]]
            for s in range(6):
                nc_ = 512 if s < 5 else 256
                (wg,), bwg = slab_load([(0, 8, nc_, wb["f_in"][:, s * 512:s * 512 + nc_])])
                (wu,), bwu = slab_load([(0, 8, nc_, wb["f_in"][:, DFF + s * 512:DFF + s * 512 + nc_])])
                for f4 in range(nc_ // 128):
                    f = s * 4 + f4
                    bg, bu = (0, 1) if f % 2 == 0 else (2, 3)
                    for k in range(8):
                        self.mm(banks[bg][:], wg[:, k, f4 * 128:(f4 + 1) * 128], h1T[:, k, :], k == 0, k == 7, [bwg] + b_h1T, [b_bank[bg]])
                    for k in range(8):
                        self.mm(banks[bu][:], wu[:, k, f4 * 128:(f4 + 1) * 128], h1T[:, k, :], k == 0, k == 7, [bwu] + b_h1T, [b_bank[bu]])
                    self.act(sl[f % 2], banks[bg][:], AF.Silu, [b_bank[bg]], b_sl[f % 2])
                    self.tt(actT[:, f, :], banks[bu][:], sl[f % 2], ALU.mult, [b_bank[bu]] + b_sl[f % 2], b_act[f:f + 1])
            for s in range(6):
                nf = 4 if s < 5 else 2
                (wd,), bwd = slab_load([(0, nf, 1024, wb["f_down"][s * 512:s * 512 + nf * 128, :])])
                for f4 in range(nf):
                    f = s * 4 + f4
                    for j in range(4):
                        for hh in range(2):
                            bk = j * 2 + hh
                            self.mm(banks[bk][:], actT[:, f, j * 128:(j + 1) * 128], wd[:, f4, hh * 512:(hh + 1) * 512], f == 0, f == NF - 1,
                                    b_act[f:f + 1] + [bwd], [b_bank[bk]], inc=(f == NF - 1 or (f4 == nf - 1 and j == 3 and hh == 1)))
            fw.dma("gpsimd", lnp[:, 0:1024], lnp_d[2, :].partition_broadcast(128), writes=b_lnp)
            fw.dma("gpsimd", lnp[:, 1024:2048], lnp_d[3, :].partition_broadcast(128), writes=b_lnp)
            ot, b_ot = tv(0, 8, F32)
            for j in (3, 2, 1, 0):
                z = ot[:, (j % 2) * 1024:(j % 2 + 1) * 1024]
                b_z = b_ot[(j % 2) * 4:(j % 2) * 4 + 4]
                for hh in range(2):
                    bk = j * 2 + hh
                    self.stt(z[:, hh * 512:(hh + 1) * 512], h1[:, j, hh * 512:(hh + 1) * 512], ALPHA, banks[bk][:], ALU.mult, ALU.add, b_h1j[j] + [b_bank[bk]], b_z)
                layer_norm_tile(z, b_z, 1, z, b_z, lnp[:, 0:1024], lnp[:, 1024:2048], b_lnp)
                t0 = g * GT + j * 128
                bo = Buf(f"out_{sq}_{g}_{j}")
                fw.dma("gpsimd", out[sq, t0:t0 + 128, :], z, reads=b_z, writes=[bo], group=b_z[0])
                b_outs.append(bo)

        try:
            self.chk(0)
            for sq in range(n_seq):
                for g in range(NG):
                    pass_a_group(sq, g)
                    if sq == 0 and g == 0:
                        prepass(["w_a", "w_b", "w_out", "f_in", "f_down"])
                    self.chk(4)
                dbg_store("oaT", o_aT[:].rearrange("p q n -> p (q n)"), b_oa)
                self.chk(5)
                for g in range(NG):
                    pass_b_group(sq, g)
                    self.chk(10)
        except StopBuild:
            pass
        if "cst" in dbg_out:
            dbg_store("cst", cstf[:, 0:128], [b_const])
        fw.wait_all("gpsimd", b_outs + list(b_w.values()))
        fw.emit()
        return nc


def host_inputs(inputs):
    w_in = np.ascontiguousarray(inputs["w_in"][0])
    w_uq = np.ascontiguousarray(inputs["mla_w_uq"][0])
    w_ukv = np.ascontiguousarray(inputs["mla_w_ukv"][0])
    w_krsw = np.zeros((1024, 96), np.float32)
    kr = w_in[:, C_KR:C_KR + 32]
    w_krsw[:, 64:80] = kr[:, 16:32]
    w_krsw[:, 80:96] = kr[:, 0:16]
    w_uqsw = np.zeros((256, 768), np.float32)
    for h in range(8):
        blk = w_uq[:, h * 96 + 64:h * 96 + 96]
        w_uqsw[:, h * 96 + 64:h * 96 + 80] = blk[:, 16:32]
        w_uqsw[:, h * 96 + 80:h * 96 + 96] = blk[:, 0:16]
    w_ki4 = np.ascontiguousarray(np.tile(w_in[:, C_KI:C_KI + 32], (1, 4)))
    kv = w_ukv.reshape(256, 8, 128)
    w_ukvk = np.ascontiguousarray(kv[:, :, 0:64].reshape(256, 512))
    w_ukvv = np.ascontiguousarray(kv[:, :, 64:128].reshape(256, 512))
    gq = np.ascontiguousarray(inputs["mla_q_norm"][0].reshape(2, 128).T)
    gkv = np.ascontiguousarray(inputs["mla_kv_norm"][0].reshape(2, 128).T)
    lnp = np.ascontiguousarray(np.stack([inputs["ln1_g"][0], inputs["ln1_b"][0], inputs["ln2_g"][0], inputs["ln2_b"][0]], 0))
    lnT = np.zeros((128, 16), np.float32)
    cst = np.zeros((128, 128 * 3 + NIT + 3), np.float32)
    cst[:, 0:128] = np.eye(128, dtype=np.float32)
    ii = np.arange(128)
    cst[:, 128:256] = (ii[None, :] >= ii[:, None]).astype(np.float32)
    cst[:, 256:384] = np.where(ii[None, :] <= ii[:, None], 0.0, -1e30)
    cst[:, 384:384 + NIT + 1] = (2.0 ** -(np.arange(NIT + 1) + 1.0))[None, :]
    inv_freq = (10000.0 ** (-np.arange(16, dtype=np.float32) / 16)).astype(np.float32)
    cst[64:80, 384 + NIT + 1] = inv_freq
    cst[80:96, 384 + NIT + 1] = inv_freq
    cst[:, 384 + NIT + 2] = 1.0
    cst[64:80, 384 + NIT + 2] = -1.0
    return {
        "w_in": w_in, "w_krsw": w_krsw, "w_ki4": w_ki4, "w_uq": w_uq, "w_uqsw": w_uqsw, "w_ukvk": w_ukvk, "w_ukvv": w_ukvv,
        "w_a": np.ascontiguousarray(inputs["w_branch_a"][0]), "w_b": np.ascontiguousarray(inputs["w_branch_b"][0]),
        "w_out": np.ascontiguousarray(inputs["w_out"][0]), "f_in": np.ascontiguousarray(inputs["ffn_w_in"][0]),
        "f_down": np.ascontiguousarray(inputs["ffn_w_down"][0]), "gq": gq, "gkv": gkv, "lnp": lnp, "lnT": lnT, "cst": cst,
    }


_NC_CACHE = {}


def kernel(**inputs):
    n_cores = 8
    n_seq = 4
    inputs = {k: np.asarray(v) for k, v in inputs.items()}
    shared = host_inputs(inputs)
    if n_seq not in _NC_CACHE:
        _NC_CACHE[n_seq] = K(n_seq).build()
    nc = _NC_CACHE[n_seq]
    x = inputs["x"]
    pos = inputs["positions"].astype(np.int32)
    in_maps = []
    for c in range(n_cores):
        m = dict(shared)
        m["x"] = np.ascontiguousarray(x[c * n_seq:(c + 1) * n_seq])
        m["pos"] = np.ascontiguousarray(pos[c * n_seq:(c + 1) * n_seq])
        in_maps.append(m)
    res = run_bass_kernel_spmd(nc, in_maps, core_ids=list(range(n_cores)))
    return np.concatenate([r["out"] for r in res.results], axis=0).astype(np.float32)
```
